# Optimizing a Trainium2 kernel written in Bass

```python
import math
import jax, jax.numpy as jnp
from jax import lax
import numpy as np

D_MODEL = 1024
BATCH = 16
SEQ = 2048
DEPTH = 2

N_A = DEPTH // 2
N_B = DEPTH - N_A
N_DENSE = (DEPTH + 1) // 2
N_MOE = DEPTH // 2

PLE_DIM = 256
EPS = 1e-6

SSM_EXPAND = 2
D_INNER = SSM_EXPAND * D_MODEL
SSM_HEADDIM = 64
SSM_HEADS = D_INNER // SSM_HEADDIM
SSM_GROUPS = 4
SSM_STATE = 128
CONV_WIDTH = 4
CHUNK = 128
CONV_DIM = D_INNER + 2 * SSM_GROUPS * SSM_STATE
D_IN_PROJ = 2 * D_INNER + 2 * SSM_GROUPS * SSM_STATE + SSM_HEADS

ATT_HEADS = 16
ATT_HEADDIM = 64
ATT_WIDTH = ATT_HEADS * ATT_HEADDIM
Q_BLOCK = 128
KV_PROJ = 2 * ATT_WIDTH + ATT_HEADS

D_FF = 3584
N_EXPERTS = 8
TOP_K = 2

kernel_name = "ssd_fox_yoco_moe_ple_block"


def rmsnorm(x, w):
    x32 = x.astype(jnp.float32)
    y = x32 * lax.rsqrt(jnp.mean(x32 * x32, axis=-1, keepdims=True) + EPS)
    return y.astype(x.dtype) * w


def swiglu(u, w_gate, w_up, w_down):
    return (jax.nn.silu(u @ w_gate) * (u @ w_up)) @ w_down


def causal_dwconv(u, w, b):
    out = lax.conv_general_dilated(
        u, w[:, None, :].astype(u.dtype), window_strides=(1,),
        padding=[(CONV_WIDTH - 1, 0)],
        dimension_numbers=('NWC', 'WIO', 'NWC'),
        feature_group_count=u.shape[-1])
    return out + b


def ssd_scan(xh, dt, A, Bg, Cg):
    b, L, H, P = xh.shape
    G, N = Bg.shape[-2:]
    K = H // G
    c = L // CHUNK
    xc = (xh * dt[..., None]).reshape(b, c, CHUNK, G, K, P)
    a = jnp.moveaxis((dt * A).reshape(b, c, CHUNK, G, K), 2, -1)
    a_cs = jnp.cumsum(a, axis=-1)
    Bc = Bg.reshape(b, c, CHUNK, G, N)
    Cc = Cg.reshape(b, c, CHUNK, G, N)
    causal = jnp.tril(jnp.ones((CHUNK, CHUNK), dtype=bool))
    seg = a_cs[..., :, None] - a_cs[..., None, :]
    decay_ls = jnp.exp(jnp.where(causal, seg, -jnp.inf))
    scores = jnp.einsum('bclgn,bcsgn->bcgls', Cc, Bc)
    y_diag = jnp.einsum('bcgls,bcgkls,bcsgkp->bclgkp', scores, decay_ls, xc)
    decay_states = jnp.exp(a_cs[..., -1:] - a_cs)
    states = jnp.einsum('bclgn,bcgkl,bclgkp->bcgkpn', Bc, decay_states, xc)
    chunk_decay = jnp.exp(a_cs[..., -1])

    def step(carry, inp):
        st, dec = inp
        return carry * dec[..., None, None] + st, carry

    init = jnp.zeros((b, G, K, P, N), dtype=states.dtype)
    _, prev = lax.scan(step, init, (jnp.moveaxis(states, 1, 0), jnp.moveaxis(chunk_decay, 1, 0)))
    prev = jnp.moveaxis(prev, 0, 1)
    y_off = jnp.einsum('bclgn,bcgkpn,bcgkl->bclgkp', Cc, prev, jnp.exp(a_cs))
    return (y_diag + y_off).reshape(b, L, H, P)


def mamba2_mixer(u, w_in, conv_w, conv_b, dt_bias, a_log, d_skip, gn_w, w_out):
    b, L, _ = u.shape
    zxbcdt = u @ w_in
    z = zxbcdt[..., :D_INNER]
    xbc = zxbcdt[..., D_INNER:D_INNER + CONV_DIM]
    dt = zxbcdt[..., D_INNER + CONV_DIM:]
    xbc = jax.nn.silu(causal_dwconv(xbc, conv_w, conv_b))
    xs = xbc[..., :D_INNER]
    Bs = xbc[..., D_INNER:D_INNER + SSM_GROUPS * SSM_STATE]
    Cs = xbc[..., D_INNER + SSM_GROUPS * SSM_STATE:]
    xh = xs.reshape(b, L, SSM_HEADS, SSM_HEADDIM)
    Bg = Bs.reshape(b, L, SSM_GROUPS, SSM_STATE)
    Cg = Cs.reshape(b, L, SSM_GROUPS, SSM_STATE)
    dt = jax.nn.softplus((dt + dt_bias).astype(jnp.float32))
    A = -jnp.exp(a_log.astype(jnp.float32))
    y = ssd_scan(xh, dt, A, Bg, Cg) + xh * d_skip[:, None]
    y = y.reshape(b, L, D_INNER)
    y = rmsnorm(y * jax.nn.silu(z), gn_w)
    return (y @ w_out).astype(u.dtype)


def fox_shared_kv(h, kv_norm_w, w_kv, b_f, k_norm_w):
    b, L, _ = h.shape
    kvf = rmsnorm(h, kv_norm_w) @ w_kv
    k = rmsnorm(kvf[..., :ATT_WIDTH].reshape(b, L, ATT_HEADS, ATT_HEADDIM), k_norm_w)
    v = kvf[..., ATT_WIDTH:2 * ATT_WIDTH].reshape(b, L, ATT_HEADS, ATT_HEADDIM)
    log_f = jax.nn.log_sigmoid((kvf[..., 2 * ATT_WIDTH:] + b_f).astype(jnp.float32))
    cum = jnp.cumsum(log_f, axis=1)
    return k, v, cum


def fox_attention(u, w_q, q_norm_w, w_o, k, v, cum):
    b, L, _ = u.shape
    q = rmsnorm((u @ w_q).reshape(b, L, ATT_HEADS, ATT_HEADDIM), q_norm_w)
    nblk = L // Q_BLOCK
    qb = jnp.moveaxis(q.reshape(b, nblk, Q_BLOCK, ATT_HEADS, ATT_HEADDIM), 1, 0)
    cumT = jnp.transpose(cum, (0, 2, 1))
    cq = jnp.moveaxis(cumT.reshape(b, ATT_HEADS, nblk, Q_BLOCK), 2, 0)
    kpos = jnp.arange(L)
    scale = ATT_HEADDIM ** -0.5

    def block(args):
        qi, ci, i = args
        qpos = i * Q_BLOCK + jnp.arange(Q_BLOCK)
        logits = jnp.einsum('bqhd,bkhd->bhqk', qi, k, preferred_element_type=jnp.float32) * scale
        logits = logits + (ci[..., :, None] - cumT[..., None, :])
        logits = jnp.where(kpos[None, :] <= qpos[:, None], logits, -jnp.inf)
        probs = jax.nn.softmax(logits, axis=-1)
        return jnp.einsum('bhqk,bkhd->bqhd', probs.astype(v.dtype), v)

    o = lax.map(block, (qb, cq, jnp.arange(nblk)))
    o = jnp.moveaxis(o, 0, 1).reshape(b, L, ATT_WIDTH)
    return (o @ w_o).astype(u.dtype)


def moe_swiglu(u, w_router, w_gate, w_up, w_down):
    logits = (u @ w_router).astype(jnp.float32)
    top_vals, top_idx = lax.top_k(logits, TOP_K)
    top_w = jax.nn.softmax(top_vals, axis=-1)
    gates = jnp.sum(jax.nn.one_hot(top_idx, N_EXPERTS, dtype=jnp.float32) * top_w[..., None], axis=-2)
    out = jnp.zeros_like(u)
    for e in range(N_EXPERTS):
        out = out + gates[..., e:e + 1].astype(u.dtype) * swiglu(u, w_gate[e], w_up[e], w_down[e])
    return out


def per_layer_embedding(h, p_i, norm_w, w_gate, w_proj):
    g = jax.nn.sigmoid(rmsnorm(h, norm_w) @ w_gate)
    return (p_i @ w_proj) * g


def setup_inputs(seed: int = 0) -> dict:
    key = jax.random.key(seed)
    ks = iter(jax.random.split(key, 40))
    f32 = jnp.float32

    def nrm(shape, fan_in):
        return jax.random.normal(next(ks), shape, f32) * (fan_in ** -0.5)

    def gain(shape):
        return 1.0 + 0.05 * jax.random.normal(next(ks), shape, f32)

    x = jax.random.normal(next(ks), (BATCH, SEQ, D_MODEL), f32)
    p = jax.random.normal(next(ks), (DEPTH, BATCH, SEQ, PLE_DIM), f32)

    dt0 = jnp.exp(jax.random.uniform(next(ks), (N_A, SSM_HEADS), f32, math.log(1e-3), math.log(1e-1)))
    ssm_dt_bias = dt0 + jnp.log(-jnp.expm1(-dt0))
    ssm_a_log = jnp.log(jax.random.uniform(next(ks), (N_A, SSM_HEADS), f32, 1.0, 16.0))

    return {
        "x": x,
        "p": p,
        "ssm_norm_w": gain((N_A, D_MODEL)),
        "ssm_w_in": nrm((N_A, D_MODEL, D_IN_PROJ), D_MODEL),
        "ssm_conv_w": nrm((N_A, CONV_WIDTH, CONV_DIM), CONV_WIDTH),
        "ssm_conv_b": 0.02 * jax.random.normal(next(ks), (N_A, CONV_DIM), f32),
        "ssm_dt_bias": ssm_dt_bias,
        "ssm_a_log": ssm_a_log,
        "ssm_d": gain((N_A, SSM_HEADS)),
        "ssm_gn_w": gain((N_A, D_INNER)),
        "ssm_w_out": nrm((N_A, D_INNER, D_MODEL), D_INNER),
        "kv_norm_w": gain((D_MODEL,)),
        "w_kv": nrm((D_MODEL, KV_PROJ), D_MODEL),
        "b_f": 2.0 + 0.5 * jax.random.normal(next(ks), (ATT_HEADS,), f32),
        "k_norm_w": gain((ATT_HEADDIM,)),
        "att_norm_w": gain((N_B, D_MODEL)),
        "att_w_q": nrm((N_B, D_MODEL, ATT_WIDTH), D_MODEL),
        "q_norm_w": gain((N_B, ATT_HEADDIM)),
        "att_w_o": nrm((N_B, ATT_WIDTH, D_MODEL), ATT_WIDTH),
        "ffn_norm_w": gain((N_DENSE, D_MODEL)),
        "ffn_w_gate": nrm((N_DENSE, D_MODEL, D_FF), D_MODEL),
        "ffn_w_up": nrm((N_DENSE, D_MODEL, D_FF), D_MODEL),
        "ffn_w_down": nrm((N_DENSE, D_FF, D_MODEL), D_FF),
        "moe_norm_w": gain((N_MOE, D_MODEL)),
        "moe_w_router": nrm((N_MOE, D_MODEL, N_EXPERTS), D_MODEL),
        "moe_w_gate": nrm((N_MOE, N_EXPERTS, D_MODEL, D_FF), D_MODEL),
        "moe_w_up": nrm((N_MOE, N_EXPERTS, D_MODEL, D_FF), D_MODEL),
        "moe_w_down": nrm((N_MOE, N_EXPERTS, D_FF, D_MODEL), D_FF),
        "ple_norm_w": gain((DEPTH, D_MODEL)),
        "ple_w_gate": nrm((DEPTH, D_MODEL, D_MODEL), D_MODEL),
        "ple_w_proj": nrm((DEPTH, PLE_DIM, D_MODEL), PLE_DIM),
    }


def reference(x, p, ssm_norm_w, ssm_w_in, ssm_conv_w, ssm_conv_b, ssm_dt_bias, ssm_a_log,
              ssm_d, ssm_gn_w, ssm_w_out, kv_norm_w, w_kv, b_f, k_norm_w,
              att_norm_w, att_w_q, q_norm_w, att_w_o,
              ffn_norm_w, ffn_w_gate, ffn_w_up, ffn_w_down,
              moe_norm_w, moe_w_router, moe_w_gate, moe_w_up, moe_w_down,
              ple_norm_w, ple_w_gate, ple_w_proj):
    h = x
    k_sh = v_sh = cum_sh = None
    for i in range(DEPTH):
        if i < N_A:
            h = h + mamba2_mixer(rmsnorm(h, ssm_norm_w[i]), ssm_w_in[i], ssm_conv_w[i], ssm_conv_b[i],
                                 ssm_dt_bias[i], ssm_a_log[i], ssm_d[i], ssm_gn_w[i], ssm_w_out[i])
        else:
            if i == N_A:
                k_sh, v_sh, cum_sh = fox_shared_kv(h, kv_norm_w, w_kv, b_f, k_norm_w)
            j = i - N_A
            h = h + fox_attention(rmsnorm(h, att_norm_w[j]), att_w_q[j], q_norm_w[j], att_w_o[j],
                                  k_sh, v_sh, cum_sh)
        if i % 2 == 0:
            d = i // 2
            h = h + swiglu(rmsnorm(h, ffn_norm_w[d]), ffn_w_gate[d], ffn_w_up[d], ffn_w_down[d])
        else:
            m = i // 2
            h = h + moe_swiglu(rmsnorm(h, moe_norm_w[m]), moe_w_router[m], moe_w_gate[m],
                               moe_w_up[m], moe_w_down[m])
        h = h + per_layer_embedding(h, p[i], ple_norm_w[i], ple_w_gate[i], ple_w_proj[i])
    return h
```

```python
import contextlib
import numpy as np
import concourse.bass as bass
import concourse.mybir as mybir
from concourse.bass_utils import run_bass_kernel_spmd

F32 = mybir.dt.float32
BF16 = mybir.dt.bfloat16
I32 = mybir.dt.int32
AF = mybir.ActivationFunctionType
ALU = mybir.AluOpType
AX = mybir.AxisListType

NCORES = 8
D = 1024
L = 2048
NSEQ = 2
TOK = NSEQ * L
NCH = L // 128
DI = 2048
NH = 32
NG = 4
DINP = 5152
DFF = 3584
NE = 8
EPS = 1e-6
ENGS = ("pe", "act", "dve", "pool", "sp")


class Op:
    __slots__ = ("eng", "fn", "r", "w", "dma", "deps", "sig", "cnt", "stream")

    def __init__(self, eng, fn, r, w, dma, stream):
        self.eng, self.fn, self.r, self.w, self.dma, self.stream = eng, fn, r, w, dma, stream
        self.deps = []
        self.sig = False
        self.cnt = 0


class Sched:
    def __init__(self, nc):
        self.nc = nc
        self.ops = []
        self.last_w = {}
        self.readers = {}
        self.last_eng = {}
        self.last_stream = {}
        self.pending_barrier = {}
        self.epoch_slots = {}

    def add(self, eng, fn, r=(), w=(), dma=False, stream=None, nobar=False):
        if dma and nobar:
            stream = "nb_" + str(stream)
        elif dma:
            stream = self.epoch_slots.setdefault(stream, len(self.epoch_slots))
        op = Op(eng, fn, tuple(r), tuple(w), dma, stream)
        deps = set()
        for k in op.r:
            lw = self.last_w.get(k)
            if lw is not None:
                deps.add(lw)
        for k in op.w:
            lw = self.last_w.get(k)
            if lw is not None:
                deps.add(lw)
            for rd in self.readers.get(k, ()):
                deps.add(rd)
        for d in deps:
            if (not d.dma) and (not op.dma) and d.eng == op.eng:
                if not any(self.last_w.get(k) is d for k in op.r):
                    continue
            op.deps.append(d)
        bar = None if nobar else self.pending_barrier.pop(eng, None)
        if bar:
            for d in bar:
                if d not in op.deps and not ((not d.dma) and d.eng == eng and not op.dma):
                    op.deps.append(d)
        for k in op.r:
            self.readers.setdefault(k, []).append(op)
        for k in op.w:
            self.last_w[k] = op
            self.readers[k] = []
        self.ops.append(op)
        if nobar:
            return op
        if dma:
            self.last_stream[stream] = op
        else:
            self.last_eng[eng] = op
        return op

    def barrier(self):
        deps = list(self.last_eng.values()) + list(self.last_stream.values())
        for e in ENGS:
            self.pending_barrier[e] = list(deps)
        self.epoch_slots = {}

    def pe(self, fn, r=(), w=()):
        return self.add("pe", fn, r, w)

    def act(self, fn, r=(), w=()):
        return self.add("act", fn, r, w)

    def dve(self, fn, r=(), w=()):
        return self.add("dve", fn, r, w)

    def pool(self, fn, r=(), w=()):
        return self.add("pool", fn, r, w)

    def dma(self, fn, r=(), w=(), q="sp", stream=None, nobar=False):
        return self.add(q, fn, r, w, dma=True, stream=stream, nobar=nobar)

    def finish(self):
        nc = self.nc
        ops = self.ops
        for op in ops:
            for d in op.deps:
                d.sig = True
        streams = {}
        eng_cnt = {e: 0 for e in ENGS}
        for op in ops:
            if op.dma:
                c = streams.get(op.stream, 0) + 16
                streams[op.stream] = c
                op.cnt = c
                op.sig = True
            elif op.sig:
                eng_cnt[op.eng] += 1
                op.cnt = eng_cnt[op.eng]
        with contextlib.ExitStack() as es:
            sems = {}
            for e in ENGS:
                if e != "sp":
                    sems[("e", e)] = es.enter_context(nc.semaphore("s_" + e))
            for s in streams:
                sems[("d", s)] = es.enter_context(nc.semaphore("d_" + str(s)))
            block = es.enter_context(nc.Block())
            per_eng = {e: [o for o in ops if o.eng == e] for e in ENGS}

            def semkey(d):
                return ("d", d.stream) if d.dma else ("e", d.eng)

            def emit(e, eng_obj):
                seen = {}
                for op in per_eng[e]:
                    need = {}
                    for d in op.deps:
                        k = semkey(d)
                        if d.cnt > need.get(k, 0):
                            need[k] = d.cnt
                    for k, v in need.items():
                        if seen.get(k, 0) >= v:
                            continue
                        eng_obj.wait_ge(sems[k], v)
                        seen[k] = v
                    ins = op.fn(eng_obj)
                    if op.sig:
                        ins.then_inc(sems[semkey(op)], 16 if op.dma else 1)
                if e == "sp":
                    for s, c in streams.items():
                        if seen.get(("d", s), 0) < c:
                            eng_obj.wait_ge(sems[("d", s)], c)

            @block.tensor
            def _(eng):
                emit("pe", eng)

            @block.scalar
            def _(eng):
                emit("act", eng)

            @block.vector
            def _(eng):
                emit("dve", eng)

            @block.gpsimd
            def _(eng):
                emit("pool", eng)

            @block.sync
            def _(eng):
                emit("sp", eng)


class Builder:
    def __init__(self, stop_after=None, dbg=False):
        self.nc = bass.Bass("TRN2", target_bir_lowering=False)
        self.S = Sched(self.nc)
        self.stop_after = stop_after
        self.uid = 0
        nc = self.nc
        self.arena = nc.alloc_sbuf_tensor("arena", [128, 51200], F32).ap()
        self.aoff = 0
        self.psum = nc.alloc_psum_tensor("psum", [128, 4096], F32).ap()
        self.inputs = {}

    def reset_arena(self, keep=0):
        self.S.barrier()
        self.aoff = keep

    def sb(self, shape, dtype):
        n = int(np.prod(shape[1:]))
        words = n if dtype in (F32, I32) else (n + 1) // 2
        words = (words + 7) // 8 * 8
        ap = self.arena[:, self.aoff:self.aoff + words]
        self.aoff += words
        assert self.aoff <= 51200, "arena overflow %d" % self.aoff
        if dtype != F32:
            ap = ap.bitcast(dtype)[:, 0:n]
        else:
            ap = ap[:, 0:n]
        if len(shape) == 3:
            ap = ap.rearrange("p (a b) -> p a b", a=shape[1])
        return ap[0:shape[0]]

    def bank(self, i, dtype=F32, n=None):
        ap = self.psum[:, i * 512:(i + 1) * 512]
        if dtype != F32:
            ap = ap.bitcast(dtype)
        if n is not None:
            ap = ap[:, 0:n]
        return ap

    def dram_in(self, name, shape, dtype=F32):
        t = self.nc.dram_tensor(name, list(shape), dtype, kind="ExternalInput").ap()
        self.inputs[name] = t
        return t

    def dram_scr(self, name, shape, dtype, kind="Internal"):
        return self.nc.dram_tensor(name, list(shape), dtype, kind=kind).ap()

    def dbg(self, name, ap, rkeys):
        if not getattr(self, "debug", False):
            return
        shape = list(ap.shape)
        t = self.nc.dram_tensor("dbg_" + name, shape, ap.dtype, kind="ExternalOutput").ap()
        self.S.dma(lambda e: e.dma_start(out=t, in_=ap), r=list(rkeys), w=["dbg_" + name], stream="dbg_" + name)

    def key(self, base):
        self.uid += 1
        return "%s#%d" % (base, self.uid)

    def consts(self):
        S = self.S
        self.identf = self.sb([128, 128], F32)
        self.ident = self.sb([128, 128], BF16)
        self.tri = self.sb([128, 128], F32)
        self.trib = self.sb([128, 128], BF16)
        self.upp = self.sb([128, 128], F32)
        self.ones = self.sb([128, 128], F32)
        self.epsb = self.sb([128, 8], F32)
        identf, ident, tri, trib, upp, ones, epsb = self.identf, self.ident, self.tri, self.trib, self.upp, self.ones, self.epsb
        S.pool(lambda e: e.memset(identf, 1.0), w=["identf"])
        S.pool(lambda e: e.affine_select(out=identf, in_=identf, pattern=[[-1, 128]], compare_op=ALU.is_equal,
                                         fill=0.0, base=0, channel_multiplier=1), r=["identf"], w=["identf"])
        S.pool(lambda e: e.tensor_copy(out=ident, in_=identf), r=["identf"], w=["ident"])
        S.pool(lambda e: e.memset(tri, 1.0), w=["tri"])
        S.pool(lambda e: e.affine_select(out=tri, in_=tri, pattern=[[1, 128]], compare_op=ALU.is_ge,
                                         fill=0.0, base=0, channel_multiplier=-1), r=["tri"], w=["tri"])
        S.pool(lambda e: e.tensor_copy(out=trib, in_=tri), r=["tri"], w=["trib"])
        S.pool(lambda e: e.memset(upp, 1.0), w=["upp"])
        S.pool(lambda e: e.affine_select(out=upp, in_=upp, pattern=[[-1, 128]], compare_op=ALU.is_gt,
                                         fill=0.0, base=0, channel_multiplier=1), r=["upp"], w=["upp"])
        S.pool(lambda e: e.memset(ones, 1.0), w=["ones"])
        S.pool(lambda e: e.memset(epsb, EPS), w=["epsb"])
        self.const_end = self.aoff

    def norm_T(self, src, nchunks, nw, uT, tag, width=D, router=None):
        S = self.S
        kc = width // 128
        hb = [self.sb([128, width], F32) for _ in range(2)]
        junk = self.sb([128, width], F32)
        xnb = [self.sb([128, width], BF16) for _ in range(2)]
        st = self.sb([128, 8], F32)
        for c in range(nchunks):
            b = c % 2
            h, xn = hb[b], xnb[b]
            kh, kx = "%s_h%d" % (tag, b), "%s_xn%d" % (tag, b)
            S.dma(lambda e, h=h, c=c: e.dma_start(out=h, in_=src[c * 128:(c + 1) * 128, :]), w=[kh], stream=kh)
            ss = st[:, 2 * b:2 * b + 1]
            rs = st[:, 2 * b + 1:2 * b + 2]
            kss = "%s_ss%d" % (tag, b)
            S.dve(lambda e, ss=ss: e.memset(ss, 0.0), w=[kss])
            S.act(lambda e, h=h, ss=ss: e.activation(out=junk, in_=h, func=AF.Square, accum_out=ss), r=[kh, kss], w=[kss, tag + "_junk"])
            S.act(lambda e, ss=ss, rs=rs: e.activation(out=rs, in_=ss, func=AF.Ln, scale=1.0 / width, bias=self.epsb[:, 0:1]),
                  r=[kss, "epsb"], w=[kss + "r"])
            S.act(lambda e, rs=rs: e.activation(out=rs, in_=rs, func=AF.Exp, scale=-0.5), r=[kss + "r"], w=[kss + "r"])
            S.dve(lambda e, h=h, xn=xn, rs=rs: e.tensor_scalar(out=xn, in0=h, scalar1=rs, scalar2=None, op0=ALU.mult),
                  r=[kh, kss + "r"], w=[kx])
            if router is not None:
                router(c, h, kh, rs, kss + "r")
            for k0 in range(0, kc, 8):
                pb = 6 + ((c + k0 // 8) % 2)
                pT = self.bank(pb, BF16).rearrange("p (a b) -> p a b", a=8)
                kp = "bank%d" % pb
                for k in range(8):
                    S.pe(lambda e, k=k, k0=k0, xn=xn, pT=pT: e.transpose(out=pT[:, k, :], in_=xn[:, (k0 + k) * 128:(k0 + k + 1) * 128],
                                                                         identity=self.ident), r=[kx, "ident"], w=[kp])
                S.dve(lambda e, k0=k0, c=c, pT=pT: e.tensor_tensor(out=uT[:, k0:k0 + 8, c * 128:(c + 1) * 128], in0=pT,
                                                                   in1=nw[:, k0:k0 + 8].unsqueeze(2).to_broadcast([128, 8, 128]),
                                                                   op=ALU.mult), r=[kp, tag + "_nw"], w=[tag + "_uT%d" % c])

    def linear_tok(self, uT, kc, nchunks, W, col_tiles, epilogue, tag, ukey, wbufs=None, banks=(0, 1), widx=None):
        S = self.S
        Wv = W.rearrange("(k p) n -> p k n", p=128) if widx is None else None
        for ti, (c0, ncol) in enumerate(col_tiles):
            self.wrot = getattr(self, "wrot", 0) + 1
            wb, kw = wbufs[self.wrot % len(wbufs)]
            if widx is None:
                S.dma(lambda e, wb=wb, c0=c0, ncol=ncol: e.dma_start(out=wb[:, :, 0:ncol], in_=Wv[:, :, c0:c0 + ncol]),
                      w=[kw], q="pool", stream=kw)
                kws = [kw] * kc
            else:
                widx_ap, widx_key = widx
                kws = ["%s_%d" % (kw, k) for k in range(kc)]
                for k in range(kc):
                    S.dma(lambda e, wb=wb, c0=c0, ncol=ncol, k=k: e.indirect_dma_start(
                        out=wb[:, k, 0:ncol], out_offset=None, in_=W,
                        in_offset=bass.IndirectOffsetOnAxis(ap=widx_ap[:, k:k + 1], axis=0)),
                        r=[widx_key, "cv_d"], w=[kws[k]], q="pool", stream=kws[k])
            for c in range(nchunks):
                bi = banks[(ti * nchunks + c) % len(banks)]
                ps = self.bank(bi, F32, ncol)
                kp = "bank%d" % bi
                for k in range(kc):
                    S.pe(lambda e, k=k, c=c, wb=wb, ps=ps, ncol=ncol: e.matmul(ps, lhsT=uT[:, k, c * 128:(c + 1) * 128], rhs=wb[:, k, 0:ncol],
                                                                              start=(k == 0), stop=(k == kc - 1)),
                         r=[kws[k], ukey(c)], w=[kp])
                epilogue(ti, c, ps, kp)


def build_program(stop_after=None, debug=False):
    B = Builder(stop_after)
    B.debug = debug
    nc, S = B.nc, B.S
    x = B.dram_in("x", [TOK, D])
    p_in = B.dram_in("p", [2, TOK, 256])
    ssm_norm_w = B.dram_in("ssm_norm_w", [128, 8])
    ssm_w_in = B.dram_in("ssm_w_in", [D, DINP])
    conv_w = B.dram_in("conv_w", [4, 128, 3072])
    conv_b = B.dram_in("conv_b", [128, 3072])
    dt_bias = B.dram_in("dt_bias", [128, NH])
    a_log = B.dram_in("a_log", [128, NH])
    d_skip = B.dram_in("d_skip", [128, NH])
    gn_w = B.dram_in("gn_w", [128, 16])
    ssm_w_out = B.dram_in("ssm_w_out", [DI, D])
    out = nc.dram_tensor("out", [TOK, D], F32, kind="ExternalOutput").ap()
    sz_scr = B.dram_scr("sz_scr", [TOK, DI], F32)
    x_scr = B.dram_scr("x_scr", [TOK, DI], BF16)
    b_scr = B.dram_scr("b_scr", [TOK, 512], BF16)
    c_scr = B.dram_scr("c_scr", [TOK, 512], BF16)
    dt_scr = B.dram_scr("dt_scr", [TOK, NH], F32)

    B.consts()

    def stage_m1(s):
        B.reset_arena(B.const_end)
        t0 = s * L
        nw = B.sb([128, 8], F32)
        S.dma(lambda e: e.dma_start(out=nw, in_=ssm_norm_w), w=["m1_nw"], stream="m1_nw")
        uT = B.sb([128, 8, L], BF16)
        B.norm_T(x[t0:t0 + L, :], NCH, nw, uT, "m1")
        cwb = [B.sb([128, 512], BF16) for _ in range(4)]
        cbb = B.sb([128, 512], F32)
        dtb = B.sb([128, NH], F32)
        S.dma(lambda e: e.dma_start(out=dtb, in_=dt_bias), w=["dtb"], stream="dtb")
        xw = [[B.sb([128, 512], BF16) for _ in range(4)] for _ in range(2)]
        shm = []
        for j in range(1, 4):
            cur = B.sb([128, 128], BF16)
            prv = B.sb([128, 128], BF16)
            tmpf = B.sb([128, 128], F32)
            kk = "shm%d" % j
            S.pool(lambda e, tmpf=tmpf: e.memset(tmpf, 1.0), w=[kk + "t"])
            S.pool(lambda e, tmpf=tmpf, j=j: e.affine_select(out=tmpf, in_=tmpf, pattern=[[1, 128]], compare_op=ALU.is_equal,
                                                            fill=0.0, base=-j, channel_multiplier=-1), r=[kk + "t"], w=[kk + "t"])
            S.pool(lambda e, tmpf=tmpf, cur=cur: e.tensor_copy(out=cur, in_=tmpf), r=[kk + "t"], w=[kk + "c"])
            S.pool(lambda e, tmpf=tmpf: e.memset(tmpf, 1.0), r=[kk + "c"], w=[kk + "t"])
            S.pool(lambda e, tmpf=tmpf, j=j: e.affine_select(out=tmpf, in_=tmpf, pattern=[[1, 128]], compare_op=ALU.is_equal,
                                                            fill=0.0, base=128 - j, channel_multiplier=-1), r=[kk + "t"], w=[kk + "t"])
            S.pool(lambda e, tmpf=tmpf, prv=prv: e.tensor_copy(out=prv, in_=tmpf), r=[kk + "t"], w=[kk + "p"])
            shm.append((cur, prv, kk))
        stage = [B.sb([128, 512], F32) for _ in range(2)]
        stageb = [B.sb([128, 512], BF16) for _ in range(2)]
        wbufs = [(B.sb([128, 8, 512], BF16), "m1_wbuf%d" % i) for i in range(2)]
        ukey = lambda c: "m1_uT%d" % c

        def ep_z(ti, c, ps, kp):
            st = stage[c % 2]
            ks = "m1_stage%d" % (c % 2)
            S.act(lambda e: e.activation(out=st, in_=ps, func=AF.Silu), r=[kp], w=[ks])
            S.dma(lambda e: e.dma_start(out=sz_scr[t0 + c * 128:t0 + (c + 1) * 128, ti * 512:(ti + 1) * 512], in_=st),
                  r=[ks], w=["sz_scr"], stream=ks)
        B.linear_tok(uT, 8, NCH, ssm_w_in[:, 0:DI], [(i * 512, 512) for i in range(4)], ep_z, "m1z", ukey, wbufs)

        for ti in range(6):
            c0 = DI + ti * 512
            for k in range(4):
                S.dma(lambda e, k=k, ti=ti: e.dma_start(out=cwb[k], in_=conv_w[k, :, ti * 512:(ti + 1) * 512]),
                      w=["cwb%d" % k], q="pool", stream="cwb%d" % k)
            S.dma(lambda e, ti=ti: e.dma_start(out=cbb, in_=conv_b[:, ti * 512:(ti + 1) * 512]), w=["cbb"], stream="cbb")
            if ti < 4:
                dst, dc0 = x_scr, ti * 512
            elif ti == 4:
                dst, dc0 = b_scr, 0
            else:
                dst, dc0 = c_scr, 0

            def ep_conv(_ti, c, ps, kp, dst=dst, dc0=dc0):
                par = c % 2
                for k in range(4):
                    S.dve(lambda e, k=k: e.tensor_tensor(out=xw[par][k], in0=ps, in1=cwb[k], op=ALU.mult),
                          r=[kp, "cwb%d" % k], w=["xw%d_%d" % (par, k)])
                cps = B.bank(2 + par)
                kc2 = "bank%d" % (2 + par)
                mm = []
                mm.append((B.ident, "ident", xw[par][3], "xw%d_3" % par))
                for j in range(1, 4):
                    cur, prv, kk = shm[j - 1]
                    mm.append((cur, kk + "c", xw[par][3 - j], "xw%d_%d" % (par, 3 - j)))
                    if c > 0:
                        mm.append((prv, kk + "p", xw[1 - par][3 - j], "xw%d_%d" % (1 - par, 3 - j)))
                for i, (lh, lk, rh, rk) in enumerate(mm):
                    S.pe(lambda e, lh=lh, rh=rh, i=i: e.matmul(cps, lhsT=lh, rhs=rh, start=(i == 0), stop=(i == len(mm) - 1)),
                         r=[lk, rk], w=[kc2])
                st = stage[par]
                ks = "m1_stage%d" % par
                S.dve(lambda e: e.tensor_tensor(out=st, in0=cps, in1=cbb, op=ALU.add), r=[kc2, "cbb"], w=[ks])
                sb_ = stageb[par]
                ksb = "m1_stageb%d" % par
                S.act(lambda e: e.activation(out=sb_, in_=st, func=AF.Silu), r=[ks], w=[ksb])
                S.dma(lambda e: e.dma_start(out=dst[t0 + c * 128:t0 + (c + 1) * 128, dc0:dc0 + 512], in_=sb_),
                      r=[ksb], w=["xbc_scr"], stream=ksb)
            B.linear_tok(uT, 8, NCH, ssm_w_in[:, c0:c0 + 512], [(0, 512)], ep_conv, "m1x%d" % (ti % 2), ukey, wbufs)

        dts = B.sb([128, 2 * NH], F32)

        def ep_dt(ti, c, ps, kp):
            d_ = dts[:, (c % 2) * NH:(c % 2 + 1) * NH]
            kd = "m1_dts%d" % (c % 2)
            S.dve(lambda e: e.tensor_tensor(out=d_, in0=ps, in1=dtb, op=ALU.add), r=[kp, "dtb"], w=[kd])
            S.act(lambda e: e.activation(out=d_, in_=d_, func=AF.Exp), r=[kd], w=[kd])
            S.act(lambda e: e.activation(out=d_, in_=d_, func=AF.Ln, bias=B.ones[:, 0:1]), r=[kd, "ones"], w=[kd])
            S.dma(lambda e: e.dma_start(out=dt_scr[t0 + c * 128:t0 + (c + 1) * 128, :], in_=d_), r=[kd], w=["dt_scr"], stream=kd)
        B.linear_tok(uT, 8, NCH, ssm_w_in[:, DI + 3072:DINP], [(0, NH)], ep_dt, "m1d", ukey, wbufs)

    for s in range(NSEQ):
        stage_m1(s)

    if stop_after == "m1":
        S.finish()
        return B

    def stage_m2(s):
        B.reset_arena(B.const_end)
        t0 = s * L
        wout = B.sb([128, 16, D], BF16)
        S.dma(lambda e: e.dma_start(out=wout, in_=ssm_w_out.rearrange("(k p) n -> p k n", p=128)), w=["m2_wout"], q="pool", stream="m2_wout")
        Abc = B.sb([128, NH], F32)
        Dsk = B.sb([128, NH], F32)
        gnw = B.sb([128, 16], F32)
        S.dma(lambda e: e.dma_start(out=Abc, in_=a_log), w=["m2_A"], stream="m2_A")
        S.dma(lambda e: e.dma_start(out=Dsk, in_=d_skip), w=["m2_D"], stream="m2_D")
        S.dma(lambda e: e.dma_start(out=gnw, in_=gn_w), w=["m2_gnw"], stream="m2_gnw")
        S.act(lambda e: e.activation(out=Abc, in_=Abc, func=AF.Exp), r=["m2_A"], w=["m2_A"])
        S.dve(lambda e: e.tensor_scalar(out=Abc, in0=Abc, scalar1=-1.0, scalar2=None, op0=ALU.mult), r=["m2_A"], w=["m2_A"])
        state = B.sb([128, 4, 512], F32)
        stateb = B.sb([128, 4, 512], BF16)
        S.pool(lambda e: e.memset(state, 0.0), w=["st%d" % g for g in range(4)])
        S.pool(lambda e: e.memset(stateb, 0.0), w=["stb%d" % g for g in range(4)])
        xt = [B.sb([128, DI], BF16) for _ in range(2)]
        bt = [B.sb([128, 512], BF16) for _ in range(2)]
        ct = [B.sb([128, 512], BF16) for _ in range(2)]
        dtt = [B.sb([128, NH], F32) for _ in range(2)]
        szt = [B.sb([128, DI], F32) for _ in range(2)]
        ht = [B.sb([128, D], F32) for _ in range(2)]
        BT_2 = [B.sb([128, 4, 128], BF16) for _ in range(2)]
        CT_2 = [B.sb([128, 4, 128], BF16) for _ in range(2)]
        av_2 = [B.sb([128, NH], F32) for _ in range(2)]
        ex_2 = [B.sb([128, 96], F32) for _ in range(2)]
        xdt_2 = [B.sb([128, DI], BF16) for _ in range(2)]
        xds_2 = [B.sb([128, DI], BF16) for _ in range(2)]
        lh = [[B.sb([128, 128], F32) for _ in range(4)] for _ in range(2)]
        LT = [B.sb([128, 4, 128], BF16) for _ in range(2)]
        MT = [B.sb([128, 4, 128], BF16) for _ in range(2)]
        scT_2 = [B.sb([128, 4, 128], BF16) for _ in range(2)]
        yv_2 = [B.sb([128, DI], F32) for _ in range(2)]
        tmp1_2 = [B.sb([128, 512], F32) for _ in range(2)]
        tmp2_2 = [B.sb([128, 512], F32) for _ in range(2)]
        junk = B.sb([128, DI], F32)
        ynb_2 = [B.sb([128, DI], BF16) for _ in range(2)]
        ynT_2 = [B.sb([128, 16, 128], BF16) for _ in range(2)]
        stt_2 = [B.sb([128, 8], F32) for _ in range(2)]
        hout = [B.sb([128, D], F32) for _ in range(2)]

        def bc(ap, n):
            return ap.unsqueeze(2).to_broadcast([128, ap.shape[1], n])

        def v3(ap, a):
            return ap.rearrange("p (a b) -> p a b", a=a)

        def chunk(c):
            b = c % 2
            r0 = t0 + c * 128
            x_, b_, c_, d_, z_, h_ = xt[b], bt[b], ct[b], dtt[b], szt[b], ht[b]
            BT, CT, av, ex, xdt, xds, scT, yv, tmp1, tmp2, ynb, ynT, stt = [v[b] for v in (BT_2, CT_2, av_2, ex_2, xdt_2, xds_2, scT_2, yv_2, tmp1_2, tmp2_2, ynb_2, ynT_2, stt_2)]
            P_ = "p%d" % b
            kx, kb, kc_, kd, kz, kh = ["m2_%s%d" % (n, b) for n in ("x", "b", "c", "d", "z", "h")]
            S.dma(lambda e, x_=x_, r0=r0: e.dma_start(out=x_, in_=x_scr[r0:r0 + 128, :]), r=["xbc_scr"], w=[kx], stream=kx)
            S.dma(lambda e, b_=b_, r0=r0: e.dma_start(out=b_, in_=b_scr[r0:r0 + 128, :]), r=["xbc_scr"], w=[kb], stream=kb)
            S.dma(lambda e, c_=c_, r0=r0: e.dma_start(out=c_, in_=c_scr[r0:r0 + 128, :]), r=["xbc_scr"], w=[kc_], stream=kc_)
            S.dma(lambda e, d_=d_, r0=r0: e.dma_start(out=d_, in_=dt_scr[r0:r0 + 128, :]), r=["dt_scr"], w=[kd], stream=kd)
            S.dma(lambda e, z_=z_, r0=r0: e.dma_start(out=z_, in_=sz_scr[r0:r0 + 128, :]), r=["sz_scr"], w=[kz], stream=kz)
            S.dma(lambda e, h_=h_, r0=r0: e.dma_start(out=h_, in_=x[r0:r0 + 128, :]), w=[kh], stream=kh)
            pT = B.bank(7, BF16).rearrange("p (a b) -> p a b", a=8)
            for g in range(4):
                S.pe(lambda e, g=g, b_=b_: e.transpose(out=pT[:, g, :], in_=b_[:, g * 128:(g + 1) * 128], identity=B.ident), r=[kb, "ident"], w=["bank7"])
                S.pe(lambda e, g=g, c_=c_: e.transpose(out=pT[:, 4 + g, :], in_=c_[:, g * 128:(g + 1) * 128], identity=B.ident), r=[kc_, "ident"], w=["bank7"])
            S.act(lambda e: e.activation(out=BT, in_=pT[:, 0:4, :], func=AF.Identity), r=["bank7"], w=["m2_BT" + P_])
            S.act(lambda e: e.activation(out=CT, in_=pT[:, 4:8, :], func=AF.Identity), r=["bank7"], w=["m2_CT" + P_])
            S.dve(lambda e, d_=d_: e.tensor_tensor(out=av, in0=d_, in1=Abc, op=ALU.mult), r=[kd, "m2_A"], w=["m2_a" + P_])
            p0 = B.bank(0)
            S.pe(lambda e: e.matmul(p0[:, 0:32], lhsT=B.tri, rhs=av, start=True, stop=True), r=["tri", "m2_a" + P_], w=["bank0"])
            S.pe(lambda e: e.matmul(p0[:, 32:64], lhsT=B.upp, rhs=av, start=True, stop=True), r=["upp", "m2_a" + P_], w=["bank0"])
            S.pe(lambda e: e.matmul(p0[:, 64:96], lhsT=B.ones, rhs=av, start=True, stop=True), r=["ones", "m2_a" + P_], w=["bank0"])
            S.act(lambda e: e.activation(out=ex, in_=p0[:, 0:96], func=AF.Exp), r=["bank0"], w=["m2_ex" + P_])
            eacs, eacr, cd = ex[:, 0:32], ex[:, 32:64], ex[:, 64:96]
            S.dve(lambda e, x_=x_, d_=d_: e.tensor_tensor(out=v3(xdt, NH), in0=v3(x_, NH), in1=bc(d_, 64), op=ALU.mult), r=[kx, kd], w=["m2_xdt" + P_])
            S.dve(lambda e: e.tensor_tensor(out=v3(xds, NH), in0=v3(xdt, NH), in1=bc(eacr, 64), op=ALU.mult), r=["m2_xdt" + P_, "m2_ex" + P_], w=["m2_xds" + P_])
            p1 = B.bank(1)
            for g in range(4):
                S.pe(lambda e, g=g: e.matmul(p1[:, g * 128:(g + 1) * 128], lhsT=BT[:, g, :], rhs=CT[:, g, :], start=True, stop=True),
                     r=["m2_BT" + P_, "m2_CT" + P_], w=["bank1"])
            S.dve(lambda e: e.tensor_tensor(out=scT, in0=v3(p1, 4), in1=B.tri.unsqueeze(1).to_broadcast([128, 4, 128]), op=ALU.mult),
                  r=["bank1", "tri"], w=["m2_scT" + P_])
            for g in range(4):
                pyd = B.bank(4)
                for hg2 in range(2):
                    hg = g * 2 + hg2
                    pp = hg % 2
                    pseg = B.bank(2 + pp)
                    kseg = "bank%d" % (2 + pp)
                    for j in range(4):
                        hh = hg * 4 + j
                        S.dve(lambda e, j=j, hh=hh, pp=pp: e.tensor_scalar(out=lh[pp][j], in0=B.upp, scalar1=av[:, hh:hh + 1], scalar2=None, op0=ALU.mult),
                               r=["upp", "m2_a" + P_], w=["m2_lh%d_%d" % (pp, j)])
                        S.pe(lambda e, j=j, pp=pp, pseg=pseg: e.matmul(pseg[:, j * 128:(j + 1) * 128], lhsT=lh[pp][j], rhs=B.tri, start=True, stop=True),
                             r=["m2_lh%d_%d" % (pp, j), "tri"], w=[kseg])
                    S.act(lambda e, pp=pp, pseg=pseg: e.activation(out=LT[pp], in_=v3(pseg, 4), func=AF.Exp), r=[kseg], w=["m2_LT%d" % pp])
                    S.dve(lambda e, pp=pp, g=g: e.tensor_tensor(out=MT[pp], in0=LT[pp], in1=scT[:, g, :].unsqueeze(1).to_broadcast([128, 4, 128]), op=ALU.mult),
                          r=["m2_LT%d" % pp, "m2_scT" + P_], w=["m2_MT%d" % pp])
                    for j in range(4):
                        hh = hg * 4 + j
                        S.pe(lambda e, j=j, hh=hh, pp=pp, pyd=pyd: e.matmul(pyd[:, (hh % 8) * 64:(hh % 8 + 1) * 64], lhsT=MT[pp][:, j, :], rhs=xdt[:, hh * 64:(hh + 1) * 64],
                                                                  start=True, stop=True), r=["m2_MT%d" % pp, "m2_xdt" + P_], w=["bank4"])
                pyo = B.bank(5)
                pst = B.bank(6)
                S.pe(lambda e, g=g: e.matmul(pyo, lhsT=CT[:, g, :], rhs=stateb[:, g, :], start=True, stop=True), r=["m2_CT" + P_, "stb%d" % g], w=["bank5"])
                S.pe(lambda e, g=g, b_=b_: e.matmul(pst, lhsT=b_[:, g * 128:(g + 1) * 128], rhs=xds[:, g * 512:(g + 1) * 512], start=True, stop=True),
                     r=[kb, "m2_xds" + P_], w=["bank6"])
                S.dve(lambda e, g=g: e.tensor_tensor(out=v3(tmp1, 8), in0=v3(pyo, 8), in1=bc(eacs[:, g * 8:(g + 1) * 8], 64), op=ALU.mult),
                      r=["bank5", "m2_ex" + P_], w=["m2_tmp1" + P_])
                S.dve(lambda e, g=g: e.tensor_tensor(out=yv[:, g * 512:(g + 1) * 512], in0=tmp1, in1=pyd, op=ALU.add), r=["m2_tmp1" + P_, "bank4"], w=["m2_y%d" % g + P_])
                S.pool(lambda e, g=g, x_=x_: e.tensor_tensor(out=v3(tmp2, 8), in0=v3(x_[:, g * 512:(g + 1) * 512], 8), in1=bc(Dsk[:, g * 8:(g + 1) * 8], 64), op=ALU.mult),
                       r=[kx, "m2_D"], w=["m2_tmp2" + P_])
                S.dve(lambda e, g=g: e.tensor_tensor(out=yv[:, g * 512:(g + 1) * 512], in0=yv[:, g * 512:(g + 1) * 512], in1=tmp2, op=ALU.add),
                      r=["m2_tmp2" + P_, "m2_y%d" % g + P_], w=["m2_y%d" % g + P_])
                S.dve(lambda e, g=g: e.tensor_tensor(out=v3(state[:, g, :], 8), in0=v3(state[:, g, :], 8), in1=bc(cd[:, g * 8:(g + 1) * 8], 64), op=ALU.mult),
                      r=["st%d" % g, "m2_ex" + P_], w=["st%d" % g])
                S.dve(lambda e, g=g: e.tensor_tensor(out=state[:, g, :], in0=state[:, g, :], in1=pst, op=ALU.add), r=["st%d" % g, "bank6"], w=["st%d" % g])
                S.act(lambda e, g=g: e.activation(out=stateb[:, g, :], in_=state[:, g, :], func=AF.Identity), r=["st%d" % g], w=["stb%d" % g])
            S.dve(lambda e, z_=z_: e.tensor_tensor(out=yv, in0=yv, in1=z_, op=ALU.mult), r=[kz] + ["m2_y%d" % g + P_ for g in range(4)], w=["m2_yz" + P_])
            ss, rs = stt[:, 0:1], stt[:, 1:2]
            S.pool(lambda e: e.memset(ss, 0.0), w=["m2_ss" + P_])
            S.act(lambda e: e.activation(out=junk, in_=yv, func=AF.Square, accum_out=ss), r=["m2_yz" + P_, "m2_ss" + P_], w=["m2_ss" + P_, "m2_junk"])
            S.act(lambda e: e.activation(out=rs, in_=ss, func=AF.Ln, scale=1.0 / DI, bias=B.epsb[:, 0:1]), r=["m2_ss" + P_, "epsb"], w=["m2_rs" + P_])
            S.act(lambda e: e.activation(out=rs, in_=rs, func=AF.Exp, scale=-0.5), r=["m2_rs" + P_], w=["m2_rs" + P_])
            S.dve(lambda e: e.tensor_scalar(out=ynb, in0=yv, scalar1=rs, scalar2=None, op0=ALU.mult), r=["m2_yz" + P_, "m2_rs" + P_], w=["m2_ynb" + P_])
            for k0 in (0, 8):
                pb = 6 + (k0 // 8)
                pTn = B.bank(pb, BF16).rearrange("p (a b) -> p a b", a=8)
                for k in range(8):
                    S.pe(lambda e, k=k, k0=k0, pTn=pTn: e.transpose(out=pTn[:, k, :], in_=ynb[:, (k0 + k) * 128:(k0 + k + 1) * 128], identity=B.ident),
                         r=["m2_ynb" + P_, "ident"], w=["bank%d" % pb])
                S.dve(lambda e, k0=k0, pTn=pTn: e.tensor_tensor(out=ynT[:, k0:k0 + 8, :], in0=pTn, in1=bc(gnw[:, k0:k0 + 8], 128), op=ALU.mult),
                      r=["bank%d" % pb, "m2_gnw"], w=["m2_ynT%d" % k0 + P_])
            ho = hout[b]
            for half in range(2):
                po = B.bank(half)
                for k in range(16):
                    S.pe(lambda e, k=k, half=half, po=po: e.matmul(po, lhsT=ynT[:, k, :], rhs=wout[:, k, half * 512:(half + 1) * 512], start=(k == 0), stop=(k == 15)),
                         r=["m2_ynT0" + P_, "m2_ynT8" + P_, "m2_wout"], w=["bank%d" % half])
                S.dve(lambda e, half=half, po=po, ho=ho, h_=h_: e.tensor_tensor(out=ho[:, half * 512:(half + 1) * 512], in0=h_[:, half * 512:(half + 1) * 512], in1=po, op=ALU.add),
                      r=["bank%d" % half, kh], w=["m2_ho%d" % b])
            S.dma(lambda e, ho=ho, r0=r0: e.dma_start(out=out[r0:r0 + 128, :], in_=ho), r=["m2_ho%d" % b], w=["out"], stream="m2_ho%d" % b)

        for c in range(NCH):
            chunk(c)

    for s in range(NSEQ):
        stage_m2(s)

    if stop_after == "m2":
        S.finish()
        return B

    TT = 1024
    NTC = TT // 128

    def stage_ffn(tile, tag, norm_w_d, wg_list, wu_list, wd_list, w_router=None):
        B.reset_arena(B.const_end)
        r0 = tile * TT
        ne = len(wg_list)
        nw = B.sb([128, 8], F32)
        S.dma(lambda e: e.dma_start(out=nw, in_=norm_w_d), w=[tag + "_nw"], stream=tag + "_nw")
        uT = B.sb([128, 8, TT], BF16)
        actT = B.sb([128, 28, TT], BF16)
        acc = B.sb([128, NTC, D], F32)
        gates = B.sb([128, NTC, 8], F32)
        wb8 = [(B.sb([128, 8, 512], BF16), tag + "_wb8_%d" % i) for i in range(2)]
        wb28 = [(B.sb([128, 28, 256], BF16), tag + "_wb28_%d" % i) for i in range(2)]
        S.dma(lambda e: e.dma_start(out=acc, in_=out[r0:r0 + TT, :].rearrange("(c p) n -> p c n", p=128)), r=["out"], w=[tag + "_acc%d" % c for c in range(NTC)],
              stream=tag + "_acc")
        mark = B.aoff
        router = None
        if w_router is not None:
            wr = B.sb([128, 8, 8], F32)
            S.dma(lambda e: e.dma_start(out=wr, in_=w_router.rearrange("(k p) n -> p k n", p=128)), w=[tag + "_wr"], stream=tag + "_wr")
            xn32 = B.sb([128, D], F32)
            u32T = B.sb([128, 8, 128], F32)
            lg = B.sb([128, 8], F32)
            lg2 = B.sb([128, 8], F32)
            mk1 = B.sb([128, 8], F32)
            mk2 = B.sb([128, 8], F32)
            sm = B.sb([128, 8], F32)

            def router(c, h, kh, rs, krs):
                S.dve(lambda e: e.tensor_scalar(out=xn32, in0=h, scalar1=rs, scalar2=None, op0=ALU.mult), r=[kh, krs], w=[tag + "_xn32"])
                for half in range(2):
                    pb = 4 + half
                    pT = B.bank(pb).rearrange("p (a b) -> p a b", a=4)
                    for k in range(4):
                        kk = half * 4 + k
                        S.pe(lambda e, k=k, kk=kk, pT=pT: e.transpose(out=pT[:, k, :], in_=xn32[:, kk * 128:(kk + 1) * 128], identity=B.identf),
                             r=[tag + "_xn32", "identf"], w=["bank%d" % pb])
                    S.dve(lambda e, half=half, pT=pT: e.tensor_tensor(out=u32T[:, half * 4:(half + 1) * 4, :], in0=pT,
                                                                      in1=nw[:, half * 4:(half + 1) * 4].unsqueeze(2).to_broadcast([128, 4, 128]), op=ALU.mult),
                          r=["bank%d" % pb, tag + "_nw"], w=[tag + "_u32T%d" % half])
                pl = B.bank(3, F32, 8)
                for k in range(8):
                    S.pe(lambda e, k=k: e.matmul(pl, lhsT=u32T[:, k, :], rhs=wr[:, k, :], start=(k == 0), stop=(k == 7)),
                         r=[tag + "_u32T0", tag + "_u32T1", tag + "_wr"], w=["bank3"])
                m1, m2, dm, w1, w2 = sm[:, 0:1], sm[:, 1:2], sm[:, 2:3], sm[:, 3:4], sm[:, 4:5]
                kq = tag + "_rt"
                S.dve(lambda e: e.tensor_copy(out=lg, in_=pl), r=["bank3"], w=[kq + "lg"])
                S.dve(lambda e: e.reduce_max(out=m1, in_=lg, axis=AX.X), r=[kq + "lg"], w=[kq + "m1"])
                S.dve(lambda e: e.tensor_scalar(out=mk1, in0=lg, scalar1=m1, scalar2=None, op0=ALU.is_equal), r=[kq + "lg", kq + "m1"], w=[kq + "mk1"])
                S.dve(lambda e: e.scalar_tensor_tensor(out=lg2, in0=mk1, scalar=-1e30, in1=lg, op0=ALU.mult, op1=ALU.add), r=[kq + "mk1", kq + "lg"], w=[kq + "lg2"])
                S.dve(lambda e: e.reduce_max(out=m2, in_=lg2, axis=AX.X), r=[kq + "lg2"], w=[kq + "m2"])
                S.dve(lambda e: e.tensor_scalar(out=mk2, in0=lg2, scalar1=m2, scalar2=None, op0=ALU.is_equal), r=[kq + "lg2", kq + "m2"], w=[kq + "mk2"])
                S.dve(lambda e: e.tensor_tensor(out=dm, in0=m2, in1=m1, op=ALU.subtract), r=[kq + "m1", kq + "m2"], w=[kq + "dm"])
                S.act(lambda e: e.activation(out=dm, in_=dm, func=AF.Exp), r=[kq + "dm"], w=[kq + "dm"])
                S.dve(lambda e: e.tensor_scalar(out=w1, in0=dm, scalar1=1.0, scalar2=None, op0=ALU.add), r=[kq + "dm"], w=[kq + "w1"])
                S.dve(lambda e: e.reciprocal(out=w1, in_=w1), r=[kq + "w1"], w=[kq + "w1"])
                S.dve(lambda e: e.tensor_tensor(out=w2, in0=dm, in1=w1, op=ALU.mult), r=[kq + "dm", kq + "w1"], w=[kq + "w2"])
                S.dve(lambda e: e.tensor_scalar(out=mk1, in0=mk1, scalar1=w1, scalar2=None, op0=ALU.mult), r=[kq + "mk1", kq + "w1"], w=[kq + "mk1"])
                S.dve(lambda e, c=c: e.scalar_tensor_tensor(out=gates[:, c, :], in0=mk2, scalar=w2, in1=mk1, op0=ALU.mult, op1=ALU.add),
                      r=[kq + "mk2", kq + "w2", kq + "mk1"], w=[tag + "_gates%d" % c])
        else:
            S.pool(lambda e: e.memset(gates, 1.0), w=[tag + "_gates%d" % c for c in range(NTC)])
        B.norm_T(out[r0:r0 + TT, :], NTC, nw, uT, tag, router=router)
        B.reset_arena(mark)
        sg = B.sb([128, NTC, 512], F32)
        ab = [B.sb([128, 512], BF16) for _ in range(2)]
        ukey = lambda c: tag + "_uT%d" % c
        akey = lambda c: tag + "_actT%d" % c
        for ei in range(ne):
            for ft in range(7):
                def ep_gate(ti, c, ps, kp):
                    S.act(lambda e: e.activation(out=sg[:, c, :], in_=ps, func=AF.Silu), r=[kp], w=[tag + "_sg%d" % c])

                def ep_up(ti, c, ps, kp, ft=ft):
                    a_ = ab[c % 2]
                    ka = tag + "_ab%d" % (c % 2)
                    S.dve(lambda e: e.tensor_tensor(out=a_, in0=sg[:, c, :], in1=ps, op=ALU.mult), r=[kp, tag + "_sg%d" % c], w=[ka])
                    pb = 6 + (c % 2)
                    pT = B.bank(pb, BF16)[:, 0:512].rearrange("p (a b) -> p a b", a=4)
                    for j in range(4):
                        S.pe(lambda e, j=j: e.transpose(out=pT[:, j, :], in_=a_[:, j * 128:(j + 1) * 128], identity=B.ident), r=[ka, "ident"], w=["bank%d" % pb])
                    S.act(lambda e: e.activation(out=actT[:, ft * 4:(ft + 1) * 4, c * 128:(c + 1) * 128], in_=pT, func=AF.Identity), r=["bank%d" % pb], w=[akey(c)])
                B.linear_tok(uT, 8, NTC, wg_list[ei][:, ft * 512:(ft + 1) * 512], [(0, 512)], ep_gate, tag + "g", ukey, wb8, banks=(0, 1))
                B.linear_tok(uT, 8, NTC, wu_list[ei][:, ft * 512:(ft + 1) * 512], [(0, 512)], ep_up, tag + "u", ukey, wb8, banks=(2, 3))

            def ep_down(ti, c, ps, kp, ei=ei):
                S.dve(lambda e: e.scalar_tensor_tensor(out=acc[:, c, ti * 256:(ti + 1) * 256], in0=ps, scalar=gates[:, c, ei:ei + 1],
                                                       in1=acc[:, c, ti * 256:(ti + 1) * 256], op0=ALU.mult, op1=ALU.add),
                      r=[kp, tag + "_gates%d" % c, tag + "_acc%d" % c], w=[tag + "_acc%d" % c])
            B.linear_tok(actT, 28, NTC, wd_list[ei], [(i * 256, 256) for i in range(4)], ep_down, tag + "d", akey, wb28, banks=(4, 5))
        S.dma(lambda e: e.dma_start(out=out[r0:r0 + TT, :].rearrange("(c p) n -> p c n", p=128), in_=acc), r=[tag + "_acc%d" % c for c in range(NTC)], w=["out"],
              stream=tag + "_acc")

    ffn_norm_w = B.dram_in("ffn_norm_w", [128, 8])
    ffn_w_gate = B.dram_in("ffn_w_gate", [D, DFF])
    ffn_w_up = B.dram_in("ffn_w_up", [D, DFF])
    ffn_w_down = B.dram_in("ffn_w_down", [DFF, D])
    for tile in range(TOK // TT):
        stage_ffn(tile, "ff", ffn_norm_w, [ffn_w_gate], [ffn_w_up], [ffn_w_down])

    if stop_after == "ffn":
        S.finish()
        return B

    ple_norm_w = B.dram_in("ple_norm_w", [2, 128, 8])
    ple_w_gate = B.dram_in("ple_w_gate", [2, D, D])
    ple_w_proj = B.dram_in("ple_w_proj", [2, 256, D])

    def stage_ple(tile, li):
        tag = "pl"
        B.reset_arena(B.const_end)
        r0 = tile * TT
        nw = B.sb([128, 8], F32)
        S.dma(lambda e: e.dma_start(out=nw, in_=ple_norm_w[li]), w=[tag + "_nw"], stream=tag + "_nw")
        uT = B.sb([128, 8, TT], BF16)
        ppT = B.sb([128, 2, TT], BF16)
        acc = B.sb([128, NTC, D], F32)
        sgm = B.sb([128, NTC, 512], F32)
        wb8 = [(B.sb([128, 8, 512], BF16), tag + "_wb8_%d" % i) for i in range(2)]
        wb2 = [(B.sb([128, 2, 512], BF16), tag + "_wb2_%d" % i) for i in range(2)]
        pld = [B.sb([128, 256], F32) for _ in range(2)]
        plb = [B.sb([128, 256], BF16) for _ in range(2)]
        dl = [B.sb([128, 512], F32) for _ in range(2)]
        S.dma(lambda e: e.dma_start(out=acc, in_=out[r0:r0 + TT, :].rearrange("(c p) n -> p c n", p=128)), r=["out"], w=[tag + "_acc%d" % c for c in range(NTC)],
              stream=tag + "_acc")
        B.norm_T(out[r0:r0 + TT, :], NTC, nw, uT, tag)
        for c in range(NTC):
            b = c % 2
            kl = tag + "_pld%d" % b
            S.dma(lambda e, c=c, b=b: e.dma_start(out=pld[b], in_=p_in[li, r0 + c * 128:r0 + (c + 1) * 128, :]), w=[kl], stream=kl)
            S.dve(lambda e, b=b: e.tensor_copy(out=plb[b], in_=pld[b]), r=[kl], w=[kl + "b"])
            pT = B.bank(4 + b, BF16)[:, 0:256].rearrange("p (a b) -> p a b", a=2)
            for k in range(2):
                S.pe(lambda e, k=k, b=b, pT=pT: e.transpose(out=pT[:, k, :], in_=plb[b][:, k * 128:(k + 1) * 128], identity=B.ident), r=[kl + "b", "ident"], w=["bank%d" % (4 + b)])
            S.act(lambda e, c=c, pT=pT: e.activation(out=ppT[:, :, c * 128:(c + 1) * 128], in_=pT, func=AF.Identity), r=["bank%d" % (4 + b)], w=[tag + "_ppT%d" % c])
        ukey = lambda c: tag + "_uT%d" % c
        pkey = lambda c: tag + "_ppT%d" % c
        for ct_ in range(2):
            def ep_g(ti, c, ps, kp):
                S.act(lambda e: e.activation(out=sgm[:, c, :], in_=ps, func=AF.Sigmoid), r=[kp], w=[tag + "_sgm%d" % c])

            def ep_p(ti, c, ps, kp, ct_=ct_):
                d_ = dl[c % 2]
                kd = tag + "_dl%d" % (c % 2)
                S.dve(lambda e: e.tensor_tensor(out=d_, in0=sgm[:, c, :], in1=ps, op=ALU.mult), r=[kp, tag + "_sgm%d" % c], w=[kd])
                S.dve(lambda e: e.tensor_tensor(out=acc[:, c, ct_ * 512:(ct_ + 1) * 512], in0=acc[:, c, ct_ * 512:(ct_ + 1) * 512], in1=d_, op=ALU.add),
                      r=[kd, tag + "_acc%d" % c], w=[tag + "_acc%d" % c])
            B.linear_tok(uT, 8, NTC, ple_w_gate[li][:, ct_ * 512:(ct_ + 1) * 512], [(0, 512)], ep_g, tag + "g", ukey, wb8, banks=(0, 1))
            B.linear_tok(ppT, 2, NTC, ple_w_proj[li][:, ct_ * 512:(ct_ + 1) * 512], [(0, 512)], ep_p, tag + "p", pkey, wb2, banks=(2, 3))
        S.dma(lambda e: e.dma_start(out=out[r0:r0 + TT, :].rearrange("(c p) n -> p c n", p=128), in_=acc), r=[tag + "_acc%d" % c for c in range(NTC)], w=["out"],
              stream=tag + "_acc")

    for tile in range(TOK // TT):
        stage_ple(tile, 0)

    if stop_after == "ple0":
        S.finish()
        return B

    kv_norm_w = B.dram_in("kv_norm_w", [128, 8])
    w_kv = B.dram_in("w_kv", [D, 2064])
    b_f = B.dram_in("b_f", [128, 16])
    k_norm_w = B.dram_in("k_norm_w", [128, 64])
    att_norm_w = B.dram_in("att_norm_w", [128, 8])
    att_w_q = B.dram_in("att_w_q", [D, D])
    q_norm_w = B.dram_in("q_norm_w", [128, 64])
    att_w_o = B.dram_in("att_w_o", [D, D])

    def bc(ap, n):
        return ap.unsqueeze(2).to_broadcast([128, ap.shape[1], n])

    def v3(ap, a):
        return ap.rearrange("p (a b) -> p a b", a=a)

    moe_w_gate = B.dram_in("moe_w_gate", [NE, D, DFF])
    moe_w_up = B.dram_in("moe_w_up", [NE, D, DFF])
    moe_w_down = B.dram_in("moe_w_down", [NE, DFF, D])
    wgub = B.dram_scr("wgub", [NE * D, 7, 2, 512], BF16)
    wdb = B.dram_scr("wdb", [NE * DFF, D], BF16)
    conv_jobs = []
    for e_ in range(NE):
        conv_jobs.append((wgub[e_ * D:(e_ + 1) * D, :, 0, :], moe_w_gate[e_].rearrange("r (t n) -> r t n", n=512), "cv_g"))
        conv_jobs.append((wgub[e_ * D:(e_ + 1) * D, :, 1, :], moe_w_up[e_].rearrange("r (t n) -> r t n", n=512), "cv_u"))
        conv_jobs.append((wdb[e_ * DFF:(e_ + 1) * DFF, :], moe_w_down[e_], "cv_d"))

    def issue_conv(n):
        for _ in range(n):
            if conv_jobs:
                dst, src, key = conv_jobs.pop(0)
                S.dma(lambda e, dst=dst, src=src: e.dma_start(out=dst, in_=src), w=[key], q="pool", stream=key, nobar=True)

    def stage_attn(s):
        tag = "at"
        B.reset_arena(B.const_end)
        t0 = s * L
        KT = B.sb([128, 16, L], BF16)
        Vaug = B.sb([128, NCH * 16 * 65], BF16).rearrange("p (c h d) -> p c h d", c=NCH, h=16)
        negc = B.sb([128, NCH, 16], F32)
        c3 = B.sb([128, NCH * 48], BF16).rearrange("p (c h j) -> p c h j", c=NCH, h=16)
        nlf = B.sb([128, NCH, 16], F32)
        knw = B.sb([128, 64], F32)
        qnw = B.sb([128, 64], F32)
        bfb = B.sb([128, 16], F32)
        lnq = B.sb([128, 8], F32)
        S.dma(lambda e: e.dma_start(out=knw, in_=k_norm_w), w=["at_knw"], stream="at_knw")
        S.dma(lambda e: e.dma_start(out=qnw, in_=q_norm_w), w=["at_qnw"], stream="at_qnw")
        S.dma(lambda e: e.dma_start(out=bfb, in_=b_f), w=["at_bfb"], stream="at_bfb")
        S.pool(lambda e: e.memset(lnq, float(np.log(0.125))), w=["at_lnq"])
        S.pool(lambda e: e.memset(Vaug, 1.0), w=["at_V"])
        mark1 = B.aoff
        nwk = B.sb([128, 8], F32)
        S.dma(lambda e: e.dma_start(out=nwk, in_=kv_norm_w), w=["kv_nw"], stream="kv_nw")
        uT = B.sb([128, 8, L], BF16)
        B.norm_T(out[t0:t0 + L, :], NCH, nwk, uT, "kv")
        wb8 = [(B.sb([128, 8, 512], BF16), "kv_wb8_%d" % i) for i in range(2)]
        kaug = [B.sb([128, 8 * 67], BF16).rearrange("p (h d) -> p h d", h=8) for _ in range(2)]
        hnb = [(B.sb([128, 512], F32), B.sb([128, 512], F32), B.sb([128, 8], F32)) for _ in range(2)]
        tf = B.sb([128, 16], F32)
        carry = B.sb([128, 16], F32)
        cs32 = B.sb([128, 16], F32)
        cs32b = B.sb([128, 16], F32)
        for i in range(2):
            S.pool(lambda e, i=i: e.memset(kaug[i], 1.0), w=["kv_kaug%d" % i])
        ukey = lambda c: "kv_uT%d" % c

        def headnorm(ps, kp, nwt, nwkey, dst, dstkey, qscale, bufs, sfx):
            sq, kn32, ssk = bufs
            ksq, kss, kkn = "hn_sq" + sfx, "hn_ssk" + sfx, "hn_kn32" + sfx
            S.act(lambda e: e.activation(out=sq, in_=ps, func=AF.Square), r=[kp], w=[ksq])
            S.dve(lambda e: e.reduce_sum(out=ssk, in_=v3(sq, 8), axis=AX.X), r=[ksq], w=[kss])
            S.act(lambda e: e.activation(out=ssk, in_=ssk, func=AF.Ln, scale=1.0 / 64, bias=B.epsb[:, 0:1]), r=[kss, "epsb"], w=[kss])
            if qscale:
                S.act(lambda e: e.activation(out=ssk, in_=ssk, func=AF.Exp, scale=-0.5, bias=lnq[:, 0:1]), r=[kss, "at_lnq"], w=[kss])
            else:
                S.act(lambda e: e.activation(out=ssk, in_=ssk, func=AF.Exp, scale=-0.5), r=[kss], w=[kss])
            S.dve(lambda e: e.tensor_tensor(out=v3(kn32, 8), in0=v3(ps, 8), in1=bc(ssk, 64), op=ALU.mult), r=[kp, kss], w=[kkn])
            S.dve(lambda e: e.tensor_tensor(out=dst, in0=v3(kn32, 8), in1=nwt.unsqueeze(1).to_broadcast([128, 8, 64]), op=ALU.mult),
                  r=[kkn, nwkey], w=[dstkey])

        def ep_k(ti, c, ps, kp):
            ka = kaug[c % 2]
            kk = "kv_kaug%d" % (c % 2)
            headnorm(ps, kp, knw, "at_knw", ka[:, :, 0:64], kk, False, hnb[c % 2], "k%d" % (c % 2))
            pb = 4 + (c % 2)
            pT = B.bank(pb, BF16).rearrange("p (a b) -> p a b", a=8)
            for j in range(8):
                S.pe(lambda e, j=j: e.transpose(out=pT[0:67, j, :], in_=ka[:, j, :], identity=B.ident), r=[kk, "ident"], w=["bank%d" % pb])
            S.act(lambda e: e.activation(out=KT[0:67, ti * 8:(ti + 1) * 8, c * 128:(c + 1) * 128], in_=pT[0:67], func=AF.Identity), r=["bank%d" % pb], w=["at_KT"])
        B.linear_tok(uT, 8, NCH, w_kv[:, 0:1024], [(0, 512), (512, 512)], ep_k, "kvk", ukey, wb8, banks=(0, 1, 2, 3))

        def ep_v(ti, c, ps, kp):
            S.act(lambda e: e.activation(out=Vaug[:, c, ti * 8:(ti + 1) * 8, 0:64], in_=v3(ps, 8), func=AF.Identity), r=[kp], w=["at_V"])
        B.linear_tok(uT, 8, NCH, w_kv[:, 1024:2048], [(0, 512), (512, 512)], ep_v, "kvv", ukey, wb8, banks=(0, 1))

        def ep_f(ti, c, ps, kp):
            S.dve(lambda e: e.tensor_tensor(out=tf, in0=ps, in1=bfb, op=ALU.add), r=[kp, "at_bfb"], w=["kv_tf"])
            S.act(lambda e: e.activation(out=tf, in_=tf, func=AF.Exp, scale=-1.0), r=["kv_tf"], w=["kv_tf"])
            S.act(lambda e: e.activation(out=nlf[:, c, :], in_=tf, func=AF.Ln, bias=B.ones[:, 0:1]), r=["kv_tf", "ones"], w=["at_nlf%d" % c])
        B.linear_tok(uT, 8, NCH, w_kv[:, 2048:2064], [(0, 16)], ep_f, "kvf", ukey, wb8, banks=(0, 1))
        S.pool(lambda e: e.memset(carry, 0.0), w=["kv_carry"])
        for c in range(NCH):
            pc = B.bank(2 + (c % 2))
            kpc = "bank%d" % (2 + (c % 2))
            S.pe(lambda e, c=c, pc=pc: e.matmul(pc[:, 0:16], lhsT=B.tri, rhs=nlf[:, c, :], start=True, stop=True), r=["tri", "at_nlf%d" % c], w=[kpc])
            S.pe(lambda e, c=c, pc=pc: e.matmul(pc[:, 16:32], lhsT=B.ones, rhs=nlf[:, c, :], start=True, stop=True), r=["ones", "at_nlf%d" % c], w=[kpc])
            S.dve(lambda e, c=c, pc=pc: e.tensor_tensor(out=negc[:, c, :], in0=pc[:, 0:16], in1=carry, op=ALU.add), r=[kpc, "kv_carry"], w=["at_negc%d" % c])
            S.dve(lambda e, pc=pc: e.tensor_tensor(out=carry, in0=pc[:, 16:32], in1=carry, op=ALU.add), r=[kpc, "kv_carry"], w=["kv_carry"])
            kc3 = "at_c3_%d" % c
            S.dve(lambda e, c=c: e.tensor_scalar(out=cs32, in0=negc[:, c, :], scalar1=-1.0, scalar2=None, op0=ALU.mult), r=["at_negc%d" % c], w=["kv_cs32"])
            for j in range(3):
                S.dve(lambda e, c=c, j=j: e.tensor_copy(out=c3[:, c, :, j], in_=cs32), r=["kv_cs32"], w=[kc3])
                if j < 2:
                    S.dve(lambda e, c=c, j=j: e.tensor_copy(out=cs32b, in_=c3[:, c, :, j]), r=[kc3], w=["kv_cs32b"])
                    S.dve(lambda e: e.tensor_tensor(out=cs32, in0=cs32, in1=cs32b, op=ALU.subtract), r=["kv_cs32", "kv_cs32b"], w=["kv_cs32"])

        if s == 0:
            B.dbg("negc", negc, ["at_negc%d" % c for c in range(NCH)])
            B.dbg("nlf", nlf, ["at_nlf%d" % c for c in range(NCH)])
            B.dbg("c3", c3, ["at_c3_%d" % c for c in range(NCH)])
            B.dbg("KT", KT, ["at_KT"])
            B.dbg("Vaug", Vaug, ["at_V"])
        B.reset_arena(mark1)
        wq = B.sb([128, 8, D], BF16)
        wo = B.sb([128, 8, D], BF16)
        nwa = B.sb([128, 8], F32)
        S.dma(lambda e: e.dma_start(out=wq, in_=att_w_q.rearrange("(k p) n -> p k n", p=128)), w=["at_wq"], q="pool", stream="at_wq")
        S.dma(lambda e: e.dma_start(out=wo, in_=att_w_o.rearrange("(k p) n -> p k n", p=128)), w=["at_wo"], q="pool", stream="at_wo")
        S.dma(lambda e: e.dma_start(out=nwa, in_=att_norm_w), w=["aq_nw"], stream="aq_nw")
        mark2 = B.aoff
        def qtile(qt):
            B.reset_arena(mark2)
            issue_conv(3)
            q00 = t0 + qt * 512
            QT = B.sb([128, 16, 512], BF16)
            qaug = [B.sb([128, 16 * 67], BF16).rearrange("p (h d) -> p h d", h=16) for _ in range(2)]
            hnq = [(B.sb([128, 512], F32), B.sb([128, 512], F32), B.sb([128, 8], F32)) for _ in range(2)]
            mark3 = B.aoff
            uTq = B.sb([128, 8, 512], BF16)
            B.norm_T(out[q00:q00 + 512, :], 4, nwa, uTq, "aq")
            for m in range(4):
                qa = qaug[m % 2]
                kqa = "at_qaug%d" % (m % 2)
                for half in range(2):
                    pq = B.bank(6 + half)
                    kpq = "bank%d" % (6 + half)
                    for k in range(8):
                        S.pe(lambda e, k=k, m=m, half=half, pq=pq: e.matmul(pq, lhsT=uTq[:, k, m * 128:(m + 1) * 128], rhs=wq[:, k, half * 512:(half + 1) * 512],
                                                                            start=(k == 0), stop=(k == 7)), r=["aq_uT%d" % m, "at_wq"], w=[kpq])
                    headnorm(pq, kpq, qnw, "at_qnw", qa[:, half * 8:(half + 1) * 8, 0:64], kqa, True, hnq[half], "q%d" % half)
                S.dve(lambda e, qa=qa, m=m: e.tensor_copy(out=qa[:, :, 64:67], in_=c3[:, 4 * qt + m, :, :]), r=["at_c3_%d" % (4 * qt + m)], w=[kqa])
                for half in range(2):
                    pb = 6 + half
                    pT = B.bank(pb, BF16).rearrange("p (a b) -> p a b", a=8)
                    for j in range(8):
                        S.pe(lambda e, j=j, half=half, qa=qa, pT=pT: e.transpose(out=pT[0:67, j, :], in_=qa[:, half * 8 + j, :], identity=B.ident),
                             r=[kqa, "ident"], w=["bank%d" % pb])
                    S.act(lambda e, half=half, m=m, pT=pT: e.activation(out=QT[0:67, half * 8:(half + 1) * 8, m * 128:(m + 1) * 128], in_=pT[0:67], func=AF.Identity),
                          r=["bank%d" % pb], w=["at_QT"])
            B.reset_arena(mark3)
            PT = [B.sb([128, 512], BF16) for _ in range(2)]
            osb = B.sb([128, 4, D], BF16)
            rinv = B.sb([128, 8], F32)
            oT = B.sb([128, 8, 128], BF16)
            hq = [B.sb([128, D], F32) for _ in range(2)]
            nj = 4 * qt + 4
            steps = [(hh, j) for hh in range(16) for j in range(nj)]

            def emit_S(i):
                hh, j = steps[i]
                q0 = max(j - 4 * qt, 0) * 128
                pb = i % 2
                pst = B.bank(pb)
                pt = PT[pb]
                kpt = "at_PT%d" % pb
                S.pe(lambda e: e.matmul(pst[:, q0:512], lhsT=KT[0:67, hh, j * 128:(j + 1) * 128], rhs=QT[0:67, hh, q0:512], start=True, stop=True),
                     r=["at_KT", "at_QT"], w=["bank%d" % pb])
                S.act(lambda e: e.activation(out=pt[:, q0:512], in_=pst[:, q0:512], func=AF.Exp, bias=negc[:, j, hh:hh + 1]),
                      r=["bank%d" % pb, "at_negc%d" % j], w=[kpt])
                if j - 4 * qt >= 0:
                    S.dve(lambda e: e.tensor_tensor(out=pt[:, q0:q0 + 128], in0=pt[:, q0:q0 + 128], in1=B.trib, op=ALU.mult), r=[kpt, "trib"], w=[kpt])

            def emit_PV(i):
                hh, j = steps[i]
                m0 = max(j - 4 * qt, 0)
                pt = PT[i % 2]
                kpt = "at_PT%d" % (i % 2)
                for m in range(m0, 4):
                    po = B.bank(2 + m, F32, 65)
                    S.pe(lambda e, m=m, po=po: e.matmul(po, lhsT=pt[:, m * 128:(m + 1) * 128], rhs=Vaug[:, j, hh, :], start=(j == 0), stop=(j == 4 * qt + m)),
                         r=[kpt, "at_V"], w=["bank%d" % (2 + m)])
                if j == nj - 1:
                    for m in range(4):
                        po = B.bank(2 + m, F32, 65)
                        S.dve(lambda e, m=m, po=po: e.reciprocal(out=rinv[:, m:m + 1], in_=po[:, 64:65]), r=["bank%d" % (2 + m)], w=["at_rinv%d" % m])
                        S.dve(lambda e, m=m, po=po: e.tensor_scalar(out=osb[:, m, hh * 64:(hh + 1) * 64], in0=po[:, 0:64], scalar1=rinv[:, m:m + 1], scalar2=None, op0=ALU.mult),
                              r=["bank%d" % (2 + m), "at_rinv%d" % m], w=["at_osb%d" % m])

            emit_S(0)
            for i in range(len(steps)):
                if i + 1 < len(steps):
                    emit_S(i + 1)
                emit_PV(i)
            if s == 0 and qt == 0:
                B.dbg("QT", QT, ["at_QT"])
                B.dbg("osb", osb, ["at_osb%d" % m for m in range(4)])
                B.dbg("rinv", rinv, ["at_rinv%d" % m for m in range(4)])
                B.dbg("PT0", PT[0], ["at_PT0"])
            for m in range(4):
                r0 = q00 + m * 128
                h_ = hq[m % 2]
                khq = "at_hq%d" % (m % 2)
                S.dma(lambda e, h_=h_, r0=r0: e.dma_start(out=h_, in_=out[r0:r0 + 128, :]), r=["out"], w=[khq], stream=khq)
                pT = B.bank(6, BF16).rearrange("p (a b) -> p a b", a=8)
                for k in range(8):
                    S.pe(lambda e, k=k, m=m, pT=pT: e.transpose(out=pT[:, k, :], in_=osb[:, m, k * 128:(k + 1) * 128], identity=B.ident), r=["at_osb%d" % m, "ident"], w=["bank6"])
                S.act(lambda e, pT=pT: e.activation(out=oT, in_=pT, func=AF.Identity), r=["bank6"], w=["at_oT"])
                for half in range(2):
                    pw = B.bank(half)
                    for k in range(8):
                        S.pe(lambda e, k=k, half=half, pw=pw: e.matmul(pw, lhsT=oT[:, k, :], rhs=wo[:, k, half * 512:(half + 1) * 512], start=(k == 0), stop=(k == 7)),
                             r=["at_oT", "at_wo"], w=["bank%d" % half])
                    S.dve(lambda e, half=half, pw=pw, h_=h_: e.tensor_tensor(out=h_[:, half * 512:(half + 1) * 512], in0=h_[:, half * 512:(half + 1) * 512], in1=pw, op=ALU.add),
                          r=["bank%d" % half, khq], w=[khq])
                S.dma(lambda e, h_=h_, r0=r0: e.dma_start(out=out[r0:r0 + 128, :], in_=h_), r=[khq], w=["out"], stream=khq)

        for qt in range(4):
            qtile(qt)

    for s in range(NSEQ):
        stage_attn(s)

    if stop_after == "attn":
        S.finish()
        return B

    moe_norm_w = B.dram_in("moe_norm_w", [128, 8])
    moe_w_router = B.dram_in("moe_w_router", [D, NE])
    issue_conv(99)

    UT = 512
    UNTC = UT // 128
    NU = 24
    NSLOT = NU * UT
    u_scr = B.dram_scr("u_scr", [TOK, D], BF16)
    slot_tok = B.dram_scr("slot_tok", [NSLOT, 1], I32)
    y_slots = B.dram_scr("y_slots", [NSLOT, D], F32)
    wgu2d = wgub.rearrange("r t g n -> (r t) (g n)")
    wd2d = wdb.rearrange("r (t n) -> (r t) n", n=512)
    NC32 = TOK // 128

    B.reset_arena(B.const_end)
    slots_i = B.sb([128, 2 * NC32], I32)
    slots_f = B.sb([128, 2 * NC32], F32)
    W12 = B.sb([128, 2 * NC32], F32)
    M1 = B.sb([128, NC32, 8], F32)
    M2 = B.sb([128, NC32, 8], F32)
    POS = B.sb([128, NC32, 8], F32)
    idxg = B.sb([128, 7, NU * 8], I32)
    idxd = B.sb([128, 2, NU * 28], I32)
    nwm = B.sb([128, 8], F32)
    S.dma(lambda e: e.dma_start(out=nwm, in_=moe_norm_w), w=["mo_nw"], stream="mo_nw")
    pmark = B.aoff

    def stage_route():
        tag = "rt"
        wr = B.sb([128, 8, 8], F32)
        S.dma(lambda e: e.dma_start(out=wr, in_=moe_w_router.rearrange("(k p) n -> p k n", p=128)), w=["rt_wr"], stream="rt_wr")
        low = B.sb([128, 128], F32)
        S.pool(lambda e: e.memset(low, 1.0), w=["rt_low"])
        S.pool(lambda e: e.affine_select(out=low, in_=low, pattern=[[1, 128]], compare_op=ALU.is_gt, fill=0.0, base=0, channel_multiplier=-1),
               r=["rt_low"], w=["rt_low"])
        hb = [B.sb([128, D], F32) for _ in range(2)]
        junk = B.sb([128, D], F32)
        xnb = [B.sb([128, D], BF16) for _ in range(2)]
        xn32 = B.sb([128, D], F32)
        u32T = B.sb([128, 8, 128], F32)
        st = B.sb([128, 8], F32)
        lg = B.sb([128, 8], F32)
        lg2 = B.sb([128, 8], F32)
        msum = B.sb([128, 8], F32)
        sm = B.sb([128, 8], F32)
        carry = B.sb([128, 8], F32)
        LG = B.sb([128, NC32, 8], F32)
        LG2 = B.sb([128, NC32, 8], F32)
        MS = B.sb([128, NC32, 8], F32)
        m1a = B.sb([128, NC32], F32)
        m2a = B.sb([128, NC32], F32)
        dma_ = B.sb([128, NC32], F32)
        w1a = B.sb([128, NC32], F32)
        S.pool(lambda e: e.memset(carry, 0.0), w=["rt_carry"])
        for c in range(NC32):
            b = c % 2
            h, xn = hb[b], xnb[b]
            kh, kx = "rt_h%d" % b, "rt_xn%d" % b
            S.dma(lambda e, h=h, c=c: e.dma_start(out=h, in_=out[c * 128:(c + 1) * 128, :]), r=["out"], w=[kh], stream=kh)
            ss, rs = st[:, 2 * b:2 * b + 1], st[:, 2 * b + 1:2 * b + 2]
            kss = "rt_ss%d" % b
            S.pool(lambda e, ss=ss: e.memset(ss, 0.0), w=[kss])
            S.act(lambda e, h=h, ss=ss: e.activation(out=junk, in_=h, func=AF.Square, accum_out=ss), r=[kh, kss], w=[kss, "rt_junk"])
            S.act(lambda e, ss=ss, rs=rs: e.activation(out=rs, in_=ss, func=AF.Ln, scale=1.0 / D, bias=B.epsb[:, 0:1]), r=[kss, "epsb"], w=[kss + "r"])
            S.act(lambda e, rs=rs: e.activation(out=rs, in_=rs, func=AF.Exp, scale=-0.5), r=[kss + "r"], w=[kss + "r"])
            S.dve(lambda e, h=h, xn=xn, rs=rs: e.tensor_scalar(out=xn, in0=h, scalar1=rs, scalar2=None, op0=ALU.mult), r=[kh, kss + "r"], w=[kx])
            S.dma(lambda e, xn=xn, c=c: e.dma_start(out=u_scr[c * 128:(c + 1) * 128, :], in_=xn), r=[kx], w=["u_scr"], stream=kx)
            S.dve(lambda e, h=h, rs=rs: e.tensor_scalar(out=xn32, in0=h, scalar1=rs, scalar2=None, op0=ALU.mult), r=[kh, kss + "r"], w=["rt_xn32"])
            for half in range(2):
                pb = 4 + half
                pT = B.bank(pb).rearrange("p (a b) -> p a b", a=4)
                for k in range(4):
                    kk = half * 4 + k
                    S.pe(lambda e, k=k, kk=kk, pT=pT: e.transpose(out=pT[:, k, :], in_=xn32[:, kk * 128:(kk + 1) * 128], identity=B.identf),
                         r=["rt_xn32", "identf"], w=["bank%d" % pb])
                S.dve(lambda e, half=half, pT=pT: e.tensor_tensor(out=u32T[:, half * 4:(half + 1) * 4, :], in0=pT,
                                                                  in1=nwm[:, half * 4:(half + 1) * 4].unsqueeze(2).to_broadcast([128, 4, 128]), op=ALU.mult),
                      r=["bank%d" % pb, "mo_nw"], w=["rt_u32T%d" % half])
            pl = B.bank(3, F32, 8)
            for k in range(8):
                S.pe(lambda e, k=k: e.matmul(pl, lhsT=u32T[:, k, :], rhs=wr[:, k, :], start=(k == 0), stop=(k == 7)),
                     r=["rt_u32T0", "rt_u32T1", "rt_wr"], w=["bank3"])
            S.dve(lambda e, c=c: e.tensor_copy(out=LG[:, c, :], in_=pl), r=["bank3"], w=["rt_LG%d" % c])
        allc = range(NC32)
        kLG = ["rt_LG%d" % c for c in allc]
        kM1 = ["rt_M1_%d" % c for c in allc]
        kM2 = ["rt_M2_%d" % c for c in allc]
        kW = ["rt_W12_%d" % c for c in allc]
        W12v = W12.rearrange("p (c k) -> p c k", k=2)

        def b8(ap):
            return ap.unsqueeze(2).to_broadcast([128, NC32, 8])
        S.dve(lambda e: e.reduce_max(out=m1a, in_=LG, axis=AX.X), r=kLG, w=["rt_m1a"])
        S.dve(lambda e: e.tensor_tensor(out=M1, in0=LG, in1=b8(m1a), op=ALU.is_equal), r=kLG + ["rt_m1a"], w=kM1)
        S.dve(lambda e: e.scalar_tensor_tensor(out=LG2, in0=M1, scalar=-1e30, in1=LG, op0=ALU.mult, op1=ALU.add), r=kM1 + kLG, w=["rt_LG2"])
        S.dve(lambda e: e.reduce_max(out=m2a, in_=LG2, axis=AX.X), r=["rt_LG2"], w=["rt_m2a"])
        S.dve(lambda e: e.tensor_tensor(out=M2, in0=LG2, in1=b8(m2a), op=ALU.is_equal), r=["rt_LG2", "rt_m2a"], w=kM2)
        S.dve(lambda e: e.tensor_tensor(out=dma_, in0=m2a, in1=m1a, op=ALU.subtract), r=["rt_m1a", "rt_m2a"], w=["rt_dma"])
        S.act(lambda e: e.activation(out=dma_, in_=dma_, func=AF.Exp), r=["rt_dma"], w=["rt_dma"])
        S.dve(lambda e: e.tensor_scalar(out=w1a, in0=dma_, scalar1=1.0, scalar2=None, op0=ALU.add), r=["rt_dma"], w=["rt_w1a"])
        S.dve(lambda e: e.reciprocal(out=w1a, in_=w1a), r=["rt_w1a"], w=["rt_w1a"])
        S.dve(lambda e: e.tensor_copy(out=W12v[:, :, 0], in_=w1a), r=["rt_w1a"], w=kW)
        S.dve(lambda e: e.tensor_tensor(out=W12v[:, :, 1], in0=dma_, in1=w1a, op=ALU.mult), r=["rt_dma", "rt_w1a"] + kW, w=kW)
        S.dve(lambda e: e.tensor_tensor(out=MS, in0=M1, in1=M2, op=ALU.add), r=kM1 + kM2, w=["rt_MS"])
        for c in range(NC32):
            pb = 2 + (c % 2)
            pp = B.bank(pb)
            S.pe(lambda e, c=c, pp=pp: e.matmul(pp[:, 0:8], lhsT=low, rhs=MS[:, c, :], start=True, stop=True), r=["rt_low", "rt_MS"], w=["bank%d" % pb])
            S.pe(lambda e, c=c, pp=pp: e.matmul(pp[:, 8:16], lhsT=B.ones, rhs=MS[:, c, :], start=True, stop=True), r=["ones", "rt_MS"], w=["bank%d" % pb])
            S.dve(lambda e, c=c, pp=pp: e.tensor_tensor(out=POS[:, c, :], in0=pp[:, 0:8], in1=carry, op=ALU.add), r=["bank%d" % pb, "rt_carry"], w=["rt_POS_%d" % c])
            S.dve(lambda e, pp=pp: e.tensor_tensor(out=carry, in0=pp[:, 8:16], in1=carry, op=ALU.add), r=["bank%d" % pb, "rt_carry"], w=["rt_carry"])
        tcnt = B.sb([128, 8], F32)
        rmod = B.sb([128, 8], F32)
        padded = B.sb([128, 8], F32)
        base = B.sb([128, 8], F32)
        bend = B.sb([128, 8], F32)
        tmp8 = B.sb([128, 8], F32)
        eu = B.sb([128, NU], F32)
        S.dve(lambda e: e.tensor_scalar(out=tcnt, in0=carry, scalar1=float(UT - 1), scalar2=None, op0=ALU.add), r=["rt_carry"], w=["rt_tcnt"])
        ri = B.sb([128, 8], I32)
        S.dve(lambda e: e.tensor_scalar(out=tcnt, in0=tcnt, scalar1=1.0 / UT, scalar2=None, op0=ALU.mult), r=["rt_tcnt"], w=["rt_tcnt"])
        S.dve(lambda e: e.tensor_copy(out=ri, in_=tcnt), r=["rt_tcnt"], w=["rt_ri"])
        S.dve(lambda e: e.tensor_copy(out=rmod, in_=ri), r=["rt_ri"], w=["rt_rmod"])
        S.dve(lambda e: e.tensor_tensor(out=padded, in0=rmod, in1=tcnt, op=ALU.is_gt), r=["rt_tcnt", "rt_rmod"], w=["rt_padded"])
        S.dve(lambda e: e.tensor_tensor(out=padded, in0=rmod, in1=padded, op=ALU.subtract), r=["rt_rmod", "rt_padded"], w=["rt_padded"])
        S.dve(lambda e: e.tensor_scalar(out=padded, in0=padded, scalar1=float(UT), scalar2=None, op0=ALU.mult), r=["rt_padded"], w=["rt_padded"])
        S.pool(lambda e: e.memset(base, 0.0), w=["rt_base"])
        for ei in range(1, 8):
            S.dve(lambda e, ei=ei: e.tensor_tensor(out=base[:, ei:ei + 1], in0=base[:, ei - 1:ei], in1=padded[:, ei - 1:ei], op=ALU.add),
                  r=["rt_base", "rt_padded"], w=["rt_base"])
        S.dve(lambda e: e.tensor_tensor(out=bend, in0=base, in1=padded, op=ALU.add), r=["rt_base", "rt_padded"], w=["rt_bend"])
        for c in range(NC32):
            for k, MM in enumerate((M1, M2)):
                S.dve(lambda e, c=c: e.tensor_tensor(out=tmp8, in0=POS[:, c, :], in1=base, op=ALU.add), r=["rt_POS_%d" % c, "rt_base"], w=["rt_tmp8"])
                S.dve(lambda e, c=c, MM=MM: e.tensor_tensor(out=tmp8, in0=tmp8, in1=MM[:, c, :], op=ALU.mult), r=["rt_tmp8", "rt_M%d_%d" % (k + 1, c)], w=["rt_tmp8"])
                S.dve(lambda e, c=c, k=k: e.reduce_sum(out=slots_f[:, 2 * c + k:2 * c + k + 1], in_=tmp8, axis=AX.X), r=["rt_tmp8"], w=["rt_slots_f"])
        S.dve(lambda e: e.tensor_copy(out=slots_i, in_=slots_f), r=["rt_slots_f"], w=["mo_slots"])
        for u in range(NU):
            S.dve(lambda e, u=u: e.tensor_scalar(out=tmp8, in0=bend, scalar1=float(UT * u), scalar2=None, op0=ALU.is_le), r=["rt_bend"], w=["rt_tmp8"])
            S.dve(lambda e, u=u: e.reduce_sum(out=eu[:, u:u + 1], in_=tmp8, axis=AX.X), r=["rt_tmp8"], w=["rt_eu"])
        S.dve(lambda e: e.tensor_scalar(out=eu, in0=eu, scalar1=7.0, scalar2=None, op0=ALU.min), r=["rt_eu"], w=["rt_eu"])
        ioi = B.sb([128, 28], I32)
        iof = B.sb([128, 28], F32)
        eus = B.sb([128, NU], F32)
        idxg_f = B.sb([128, NU * 8], F32)
        idxd_f = B.sb([128, NU * 28], F32)
        S.pool(lambda e: e.iota(ioi, pattern=[[128, 28]], base=0, channel_multiplier=1), w=["rt_ioi"])
        S.dve(lambda e: e.tensor_copy(out=iof, in_=ioi), r=["rt_ioi"], w=["rt_iof"])
        S.dve(lambda e: e.tensor_scalar(out=eus, in0=eu, scalar1=float(D), scalar2=None, op0=ALU.mult), r=["rt_eu"], w=["rt_eus"])
        for u in range(NU):
            S.dve(lambda e, u=u: e.tensor_scalar(out=idxg_f[:, u * 8:(u + 1) * 8], in0=iof[:, 0:8], scalar1=eus[:, u:u + 1], scalar2=None, op0=ALU.add),
                  r=["rt_iof", "rt_eus"], w=["rt_idxg_f"])
        tmpi = B.sb([128, NU * 28], F32)
        for ft in range(7):
            S.dve(lambda e, ft=ft: e.tensor_scalar(out=tmpi[:, 0:NU * 8], in0=idxg_f, scalar1=7.0, scalar2=float(ft), op0=ALU.mult, op1=ALU.add),
                  r=["rt_idxg_f"], w=["rt_tmpi"])
            S.dve(lambda e, ft=ft: e.tensor_copy(out=idxg[:, ft, :], in_=tmpi[:, 0:NU * 8]), r=["rt_tmpi"], w=["mo_idxg"])
        S.dve(lambda e: e.tensor_scalar(out=eus, in0=eu, scalar1=float(DFF), scalar2=None, op0=ALU.mult), r=["rt_eu", "rt_idxg_f"], w=["rt_eus"])
        for u in range(NU):
            S.dve(lambda e, u=u: e.tensor_scalar(out=idxd_f[:, u * 28:(u + 1) * 28], in0=iof, scalar1=eus[:, u:u + 1], scalar2=None, op0=ALU.add),
                  r=["rt_iof", "rt_eus"], w=["rt_idxd_f"])
        for ti in range(2):
            S.dve(lambda e, ti=ti: e.tensor_scalar(out=tmpi, in0=idxd_f, scalar1=2.0, scalar2=float(ti), op0=ALU.mult, op1=ALU.add),
                  r=["rt_idxd_f"], w=["rt_tmpi"])
            S.dve(lambda e, ti=ti: e.tensor_copy(out=idxd[:, ti, :], in_=tmpi), r=["rt_tmpi"], w=["mo_idxd"])
        zi = B.sb([128, NSLOT // 128], I32)
        tokid = B.sb([128, NC32], I32)
        S.pool(lambda e: e.memset(zi, 0), w=["rt_zi"])
        S.pool(lambda e: e.iota(tokid, pattern=[[128, NC32]], base=0, channel_multiplier=1), w=["rt_tokid"])
        S.dma(lambda e: e.dma_start(out=slot_tok.rearrange("(p a) o -> p (a o)", p=128), in_=zi), r=["rt_zi"], w=["slot_tok"], stream="rt_zi")
        for c in range(NC32):
            for k in range(2):
                S.dma(lambda e, c=c, k=k: e.indirect_dma_start(out=slot_tok, out_offset=bass.IndirectOffsetOnAxis(ap=slots_i[:, 2 * c + k:2 * c + k + 1], axis=0),
                                                               in_=tokid[:, c:c + 1], in_offset=None),
                      r=["mo_slots", "rt_tokid"], w=["slot_tok"], q="pool", stream="rt_sct")
        if B.debug:
            B.dbg("slots_i", slots_i, ["mo_slots"])
            B.dbg("W12", W12, ["rt_W12_%d" % c for c in range(NC32)])
            B.dbg("eu", eu, ["rt_eu"])
            B.dbg("idxg", idxg, ["mo_idxg"])
            B.dbg("idxd", idxd, ["mo_idxd"])

    stage_route()

    B.reset_arena(pmark)
    mu_uT = [B.sb([128, 8, UT], BF16) for _ in range(2)]
    mu_actT = B.sb([128, 28, UT], BF16)
    mu_acc = [B.sb([128, UNTC, D], F32) for _ in range(2)]
    mu_wgu = [B.sb([128, 8, 1024], BF16) for _ in range(2)]
    mu_wb28 = [(B.sb([128, 28, 512], BF16), "mu_wb28_%d" % i) for i in range(2)]
    mu_sg = [B.sb([128, 512], F32) for _ in range(2)]
    mu_xg = [B.sb([128, D], BF16) for _ in range(2)]
    mu_sidx = [B.sb([128, UNTC], I32) for _ in range(2)]

    def unit_pre(u):
        par = u % 2
        r0 = u * UT
        uT, sidx = mu_uT[par], mu_sidx[par]
        ksi = "mu_sidx%d" % par
        S.dma(lambda e: e.dma_start(out=sidx, in_=slot_tok[r0:r0 + UT, :].rearrange("(c p) o -> p (c o)", p=128), allow_slow_non_contiguous=True),
              r=["slot_tok"], w=[ksi], stream=ksi)
        ukeys = ["mu_uT%d_%d" % (par, c) for c in range(UNTC)]
        for c in range(UNTC):
            b = c % 2
            kx = "mu_xg%d" % b
            S.dma(lambda e, c=c, b=b: e.indirect_dma_start(out=mu_xg[b], out_offset=None, in_=u_scr, in_offset=bass.IndirectOffsetOnAxis(ap=sidx[:, c:c + 1], axis=0)),
                  r=[ksi, "u_scr"], w=[kx], q="pool", stream=kx)
            pb = 6 + b
            pT = B.bank(pb, BF16).rearrange("p (a b) -> p a b", a=8)
            for k in range(8):
                S.pe(lambda e, k=k, b=b, pT=pT: e.transpose(out=pT[:, k, :], in_=mu_xg[b][:, k * 128:(k + 1) * 128], identity=B.ident), r=[kx, "ident"], w=["bank%d" % pb])
            S.dve(lambda e, c=c, pT=pT: e.tensor_tensor(out=uT[:, :, c * 128:(c + 1) * 128], in0=pT, in1=nwm.unsqueeze(2).to_broadcast([128, 8, 128]), op=ALU.mult),
                  r=["bank%d" % pb, "mo_nw"], w=[ukeys[c]])

    def unit_gateup(u):
        par = u % 2
        uT = mu_uT[par]
        actT = mu_actT
        ukeys = ["mu_uT%d_%d" % (par, c) for c in range(UNTC)]
        for ft in range(7):
            wgu = mu_wgu[ft % 2]
            wb_g, wb_u = wgu[:, :, 0:512], wgu[:, :, 512:1024]
            for k in range(8):
                kk = "mu_wgu%d_%d" % (ft % 2, k)
                S.dma(lambda e, wgu=wgu, k=k, ft=ft: e.indirect_dma_start(
                    out=wgu[:, k, :], out_offset=None, in_=wgu2d,
                    in_offset=bass.IndirectOffsetOnAxis(ap=idxg[:, ft, u * 8 + k:u * 8 + k + 1], axis=0)),
                    r=["mo_idxg", "cv_g", "cv_u"], w=[kk], q="pool", stream=kk)
            for j in range(4):
                fc = ft * 4 + j
                pg = B.bank(fc % 2)
                pu = B.bank(2 + fc % 2)
                for k in range(8):
                    S.pe(lambda e, k=k, j=j, pg=pg, wb_g=wb_g: e.matmul(pg, lhsT=wb_g[:, k, j * 128:(j + 1) * 128], rhs=uT[:, k, :], start=(k == 0), stop=(k == 7)),
                         r=["mu_wgu%d_%d" % (ft % 2, k)] + ukeys, w=["bank%d" % (fc % 2)])
                for k in range(8):
                    S.pe(lambda e, k=k, j=j, pu=pu, wb_u=wb_u: e.matmul(pu, lhsT=wb_u[:, k, j * 128:(j + 1) * 128], rhs=uT[:, k, :], start=(k == 0), stop=(k == 7)),
                         r=["mu_wgu%d_%d" % (ft % 2, k)] + ukeys, w=["bank%d" % (2 + fc % 2)])
                sg_ = mu_sg[fc % 2]
                ksg = "mu_sg%d" % (fc % 2)
                S.act(lambda e, pg=pg, sg_=sg_: e.activation(out=sg_, in_=pg, func=AF.Silu), r=["bank%d" % (fc % 2)], w=[ksg])
                S.dve(lambda e, fc=fc, pu=pu, sg_=sg_: e.tensor_tensor(out=actT[:, fc, :], in0=sg_, in1=pu, op=ALU.mult), r=[ksg, "bank%d" % (2 + fc % 2)], w=["mu_actT"])

    def unit_down(u):
        tag = "mu"
        par = u % 2
        r0 = u * UT
        acc = mu_acc[par]
        actT = mu_actT
        akey = lambda c: "mu_actT"
        for ti4 in range(2):
            def ep_down(ti, c, ps, kp, ti4=ti4):
                S.act(lambda e: e.activation(out=acc[:, c, ti4 * 512:(ti4 + 1) * 512], in_=ps, func=AF.Identity), r=[kp], w=["mu_acc%d_%d" % (par, c)])
            idn = (idxd[:, ti4, u * 28:(u + 1) * 28], "mo_idxd")
            B.linear_tok(actT, 28, UNTC, wd2d, [(0, 512)], ep_down, tag + "d", akey, mu_wb28, banks=(4, 5), widx=idn)
        S.dma(lambda e: e.dma_start(out=y_slots[r0:r0 + UT, :].rearrange("(c p) n -> p c n", p=128), in_=acc), r=["mu_acc%d_%d" % (par, c) for c in range(UNTC)], w=["y_slots"],
              stream="mu_accst%d" % par)

    unit_pre(0)
    for u in range(NU):
        unit_gateup(u)
        if u + 1 < NU:
            unit_pre(u + 1)
        unit_down(u)

    def stage_combine():
        tag = "cb"
        B.reset_arena(pmark)
        hb = [B.sb([128, D], F32) for _ in range(2)]
        y1 = [B.sb([128, D], F32) for _ in range(2)]
        y2 = [B.sb([128, D], F32) for _ in range(2)]
        for c in range(NC32):
            b = c % 2
            kh, k1, k2 = "cb_h%d" % b, "cb_y1%d" % b, "cb_y2%d" % b
            S.dma(lambda e, b=b, c=c: e.dma_start(out=hb[b], in_=out[c * 128:(c + 1) * 128, :]), r=["out"], w=[kh], stream=kh)
            S.dma(lambda e, b=b, c=c: e.indirect_dma_start(out=y1[b], out_offset=None, in_=y_slots, in_offset=bass.IndirectOffsetOnAxis(ap=slots_i[:, 2 * c:2 * c + 1], axis=0)),
                  r=["y_slots", "mo_slots"], w=[k1], q="pool", stream=k1)
            S.dma(lambda e, b=b, c=c: e.indirect_dma_start(out=y2[b], out_offset=None, in_=y_slots, in_offset=bass.IndirectOffsetOnAxis(ap=slots_i[:, 2 * c + 1:2 * c + 2], axis=0)),
                  r=["y_slots", "mo_slots"], w=[k2], q="pool", stream=k2)
            S.dve(lambda e, b=b, c=c: e.scalar_tensor_tensor(out=hb[b], in0=y1[b], scalar=W12[:, 2 * c:2 * c + 1], in1=hb[b], op0=ALU.mult, op1=ALU.add),
                  r=[k1, kh, "rt_W12_%d" % c], w=[kh])
            S.dve(lambda e, b=b, c=c: e.scalar_tensor_tensor(out=hb[b], in0=y2[b], scalar=W12[:, 2 * c + 1:2 * c + 2], in1=hb[b], op0=ALU.mult, op1=ALU.add),
                  r=[k2, kh, "rt_W12_%d" % c], w=[kh])
            S.dma(lambda e, b=b, c=c: e.dma_start(out=out[c * 128:(c + 1) * 128, :], in_=hb[b]), r=[kh], w=["out"], stream=kh)

    stage_combine()
    if stop_after == "moe":
        S.finish()
        return B
    for tile in range(TOK // TT):
        stage_ple(tile, 1)

    S.finish()
    return B


def _rep(v, n=128):
    return np.ascontiguousarray(np.broadcast_to(np.asarray(v, np.float32)[None], (n,) + tuple(np.shape(v))))


def _col(v):
    v = np.asarray(v, np.float32)
    return np.ascontiguousarray(v.reshape(-1, 128).T)


def prep_inputs(inp, core):
    b0 = core * NSEQ
    m = {}
    m["x"] = np.ascontiguousarray(inp["x"][b0:b0 + NSEQ].reshape(TOK, D))
    m["p"] = np.ascontiguousarray(inp["p"][:, b0:b0 + NSEQ].reshape(2, TOK, 256))
    m["ssm_norm_w"] = _col(inp["ssm_norm_w"][0])
    m["ssm_w_in"] = np.ascontiguousarray(inp["ssm_w_in"][0])
    m["conv_w"] = np.ascontiguousarray(np.broadcast_to(np.asarray(inp["ssm_conv_w"][0], np.float32)[:, None, :], (4, 128, 3072)))
    m["conv_b"] = _rep(inp["ssm_conv_b"][0])
    m["dt_bias"] = _rep(inp["ssm_dt_bias"][0])
    m["a_log"] = _rep(inp["ssm_a_log"][0])
    m["d_skip"] = _rep(inp["ssm_d"][0])
    m["gn_w"] = _col(inp["ssm_gn_w"][0])
    m["ssm_w_out"] = np.ascontiguousarray(inp["ssm_w_out"][0])
    m["ffn_norm_w"] = _col(inp["ffn_norm_w"][0])
    m["moe_norm_w"] = _col(inp["moe_norm_w"][0])
    m["moe_w_router"] = np.ascontiguousarray(inp["moe_w_router"][0])
    m["moe_w_gate"] = np.ascontiguousarray(inp["moe_w_gate"][0])
    m["moe_w_up"] = np.ascontiguousarray(inp["moe_w_up"][0])
    m["moe_w_down"] = np.ascontiguousarray(inp["moe_w_down"][0])
    m["kv_norm_w"] = _col(inp["kv_norm_w"])
    m["w_kv"] = np.ascontiguousarray(inp["w_kv"])
    m["b_f"] = _rep(inp["b_f"])
    m["k_norm_w"] = _rep(inp["k_norm_w"])
    m["att_norm_w"] = _col(inp["att_norm_w"][0])
    m["att_w_q"] = np.ascontiguousarray(inp["att_w_q"][0])
    m["q_norm_w"] = _rep(inp["q_norm_w"][0])
    m["att_w_o"] = np.ascontiguousarray(inp["att_w_o"][0])
    m["ple_norm_w"] = np.stack([_col(inp["ple_norm_w"][i]) for i in range(2)])
    m["ple_w_gate"] = np.ascontiguousarray(inp["ple_w_gate"])
    m["ple_w_proj"] = np.ascontiguousarray(inp["ple_w_proj"])
    m["ffn_w_gate"] = np.ascontiguousarray(inp["ffn_w_gate"][0])
    m["ffn_w_up"] = np.ascontiguousarray(inp["ffn_w_up"][0])
    m["ffn_w_down"] = np.ascontiguousarray(inp["ffn_w_down"][0])
    return m


def kernel(**inputs):
    inp = {k: np.asarray(v) for k, v in inputs.items()}
    B = build_program()
    in_maps = []
    for c in range(NCORES):
        m = prep_inputs(inp, c)
        in_maps.append({k: v for k, v in m.items() if k in B.inputs})
    res = run_bass_kernel_spmd(B.nc, in_maps, core_ids=list(range(NCORES)))
    outs = [np.asarray(r["out"]).reshape(NSEQ, L, D) for r in res.results]
    return np.concatenate(outs, axis=0).astype(np.float32)
```

```python
import contextlib
import numpy as np
import concourse.bass as bass
import concourse.mybir as mybir
from concourse.bass_utils import run_bass_kernel_spmd

F32 = mybir.dt.float32
BF16 = mybir.dt.bfloat16
I32 = mybir.dt.int32
AF = mybir.ActivationFunctionType
ALU = mybir.AluOpType
AX = mybir.AxisListType

NCORES = 8
D = 1024
L = 2048
NSEQ = 2
TOK = NSEQ * L
NCH = L // 128
DI = 2048
NH = 32
NG = 4
DINP = 5152
DFF = 3584
NE = 8
EPS = 1e-6
ENGS = ("pe", "act", "dve", "pool", "sp")


class Op:
    __slots__ = ("eng", "fn", "r", "w", "dma", "deps", "sig", "cnt", "stream")

    def __init__(self, eng, fn, r, w, dma, stream):
        self.eng, self.fn, self.r, self.w, self.dma, self.stream = eng, fn, r, w, dma, stream
        self.deps = []
        self.sig = False
        self.cnt = 0


class Sched:
    def __init__(self, nc):
        self.nc = nc
        self.ops = []
        self.last_w = {}
        self.readers = {}
        self.last_eng = {}
        self.last_stream = {}
        self.pending_barrier = {}
        self.epoch_slots = {}

    def add(self, eng, fn, r=(), w=(), dma=False, stream=None, nobar=False):
        if dma and nobar:
            stream = "nb_" + str(stream)
        elif dma:
            cls = "sw" if eng == "pool" else "hw"
            slots = self.epoch_slots.setdefault(cls, {})
            stream = cls + str(slots.setdefault(stream, len(slots)))
        op = Op(eng, fn, tuple(r), tuple(w), dma, stream)
        deps = set()
        for k in op.r:
            lw = self.last_w.get(k)
            if lw is not None:
                deps.add(lw)
        for k in op.w:
            lw = self.last_w.get(k)
            if lw is not None:
                deps.add(lw)
            for rd in self.readers.get(k, ()):
                deps.add(rd)
        for d in deps:
            if (not d.dma) and (not op.dma) and d.eng == op.eng:
                if not any(self.last_w.get(k) is d for k in op.r):
                    continue
            op.deps.append(d)
        bar = None if nobar else self.pending_barrier.pop(eng, None)
        if bar:
            for d in bar:
                if d not in op.deps and not ((not d.dma) and d.eng == eng and not op.dma):
                    op.deps.append(d)
        for k in op.r:
            self.readers.setdefault(k, []).append(op)
        for k in op.w:
            self.last_w[k] = op
            self.readers[k] = []
        self.ops.append(op)
        if nobar:
            return op
        if dma:
            self.last_stream[stream] = op
        else:
            self.last_eng[eng] = op
        return op

    def barrier(self):
        deps = list(self.last_eng.values()) + list(self.last_stream.values())
        for e in ENGS:
            self.pending_barrier[e] = list(deps)
        self.epoch_slots = {}

    def pe(self, fn, r=(), w=()):
        return self.add("pe", fn, r, w)

    def act(self, fn, r=(), w=()):
        return self.add("act", fn, r, w)

    def dve(self, fn, r=(), w=()):
        return self.add("dve", fn, r, w)

    def pool(self, fn, r=(), w=()):
        return self.add("pool", fn, r, w)

    def dma(self, fn, r=(), w=(), q="sp", stream=None, nobar=False):
        return self.add(q, fn, r, w, dma=True, stream=stream, nobar=nobar)

    def finish(self):
        nc = self.nc
        ops = self.ops
        for op in ops:
            for d in op.deps:
                d.sig = True
        streams = {}
        eng_cnt = {e: 0 for e in ENGS}
        for op in ops:
            if op.dma:
                c = streams.get(op.stream, 0) + 16
                streams[op.stream] = c
                op.cnt = c
                op.sig = True
            elif op.sig:
                eng_cnt[op.eng] += 1
                op.cnt = eng_cnt[op.eng]
        with contextlib.ExitStack() as es:
            sems = {}
            for e in ENGS:
                if e != "sp":
                    sems[("e", e)] = es.enter_context(nc.semaphore("s_" + e))
            for s in streams:
                sems[("d", s)] = es.enter_context(nc.semaphore("d_" + str(s)))
            block = es.enter_context(nc.Block())
            per_eng = {e: [o for o in ops if o.eng == e] for e in ENGS}

            def semkey(d):
                return ("d", d.stream) if d.dma else ("e", d.eng)

            def emit(e, eng_obj):
                seen = {}
                for op in per_eng[e]:
                    need = {}
                    for d in op.deps:
                        k = semkey(d)
                        if d.cnt > need.get(k, 0):
                            need[k] = d.cnt
                    for k, v in need.items():
                        if seen.get(k, 0) >= v:
                            continue
                        eng_obj.wait_ge(sems[k], v)
                        seen[k] = v
                    ins = op.fn(eng_obj)
                    if op.sig:
                        ins.then_inc(sems[semkey(op)], 16 if op.dma else 1)
                if e == "sp":
                    for s, c in streams.items():
                        if seen.get(("d", s), 0) < c:
                            eng_obj.wait_ge(sems[("d", s)], c)

            @block.tensor
            def _(eng):
                emit("pe", eng)

            @block.scalar
            def _(eng):
                emit("act", eng)

            @block.vector
            def _(eng):
                emit("dve", eng)

            @block.gpsimd
            def _(eng):
                emit("pool", eng)

            @block.sync
            def _(eng):
                emit("sp", eng)


class Builder:
    def __init__(self, stop_after=None, dbg=False):
        self.nc = bass.Bass("TRN2", target_bir_lowering=False)
        self.S = Sched(self.nc)
        self.stop_after = stop_after
        self.uid = 0
        nc = self.nc
        self.arena = nc.alloc_sbuf_tensor("arena", [128, 51200], F32).ap()
        self.aoff = 0
        self.psum = nc.alloc_psum_tensor("psum", [128, 4096], F32).ap()
        self.inputs = {}

    def reset_arena(self, keep=0):
        self.S.barrier()
        self.aoff = keep

    def sb(self, shape, dtype):
        n = int(np.prod(shape[1:]))
        words = n if dtype in (F32, I32) else (n + 1) // 2
        words = (words + 7) // 8 * 8
        ap = self.arena[:, self.aoff:self.aoff + words]
        self.aoff += words
        assert self.aoff <= 51200, "arena overflow %d" % self.aoff
        if dtype != F32:
            ap = ap.bitcast(dtype)[:, 0:n]
        else:
            ap = ap[:, 0:n]
        if len(shape) == 3:
            ap = ap.rearrange("p (a b) -> p a b", a=shape[1])
        return ap[0:shape[0]]

    def bank(self, i, dtype=F32, n=None):
        ap = self.psum[:, i * 512:(i + 1) * 512]
        if dtype != F32:
            ap = ap.bitcast(dtype)
        if n is not None:
            ap = ap[:, 0:n]
        return ap

    def dram_in(self, name, shape, dtype=F32):
        t = self.nc.dram_tensor(name, list(shape), dtype, kind="ExternalInput").ap()
        self.inputs[name] = t
        return t

    def dram_scr(self, name, shape, dtype, kind="Internal"):
        return self.nc.dram_tensor(name, list(shape), dtype, kind=kind).ap()

    def dbg(self, name, ap, rkeys):
        if not getattr(self, "debug", False):
            return
        shape = list(ap.shape)
        t = self.nc.dram_tensor("dbg_" + name, shape, ap.dtype, kind="ExternalOutput").ap()
        self.S.dma(lambda e: e.dma_start(out=t, in_=ap), r=list(rkeys), w=["dbg_" + name], stream="dbg_" + name)

    def key(self, base):
        self.uid += 1
        return "%s#%d" % (base, self.uid)

    def consts(self):
        S = self.S
        self.identf = self.sb([128, 128], F32)
        self.ident = self.sb([128, 128], BF16)
        self.tri = self.sb([128, 128], F32)
        self.trib = self.sb([128, 128], BF16)
        self.upp = self.sb([128, 128], F32)
        self.ones = self.sb([128, 128], F32)
        self.epsb = self.sb([128, 8], F32)
        identf, ident, tri, trib, upp, ones, epsb = self.identf, self.ident, self.tri, self.trib, self.upp, self.ones, self.epsb
        S.pool(lambda e: e.memset(identf, 1.0), w=["identf"])
        S.pool(lambda e: e.affine_select(out=identf, in_=identf, pattern=[[-1, 128]], compare_op=ALU.is_equal,
                                         fill=0.0, base=0, channel_multiplier=1), r=["identf"], w=["identf"])
        S.pool(lambda e: e.tensor_copy(out=ident, in_=identf), r=["identf"], w=["ident"])
        S.pool(lambda e: e.memset(tri, 1.0), w=["tri"])
        S.pool(lambda e: e.affine_select(out=tri, in_=tri, pattern=[[1, 128]], compare_op=ALU.is_ge,
                                         fill=0.0, base=0, channel_multiplier=-1), r=["tri"], w=["tri"])
        S.pool(lambda e: e.tensor_copy(out=trib, in_=tri), r=["tri"], w=["trib"])
        S.pool(lambda e: e.memset(upp, 1.0), w=["upp"])
        S.pool(lambda e: e.affine_select(out=upp, in_=upp, pattern=[[-1, 128]], compare_op=ALU.is_gt,
                                         fill=0.0, base=0, channel_multiplier=1), r=["upp"], w=["upp"])
        S.pool(lambda e: e.memset(ones, 1.0), w=["ones"])
        S.pool(lambda e: e.memset(epsb, EPS), w=["epsb"])
        self.const_end = self.aoff

    def norm_T(self, src, nchunks, nw, uT, tag, width=D, router=None):
        S = self.S
        kc = width // 128
        hb = [self.sb([128, width], F32) for _ in range(2)]
        junk = self.sb([128, width], F32)
        xnb = [self.sb([128, width], BF16) for _ in range(2)]
        st = self.sb([128, 8], F32)
        for c in range(nchunks):
            b = c % 2
            h, xn = hb[b], xnb[b]
            kh, kx = "%s_h%d" % (tag, b), "%s_xn%d" % (tag, b)
            S.dma(lambda e, h=h, c=c: e.dma_start(out=h, in_=src[c * 128:(c + 1) * 128, :]), w=[kh], stream=kh)
            ss = st[:, 2 * b:2 * b + 1]
            rs = st[:, 2 * b + 1:2 * b + 2]
            kss = "%s_ss%d" % (tag, b)
            S.dve(lambda e, ss=ss: e.memset(ss, 0.0), w=[kss])
            S.act(lambda e, h=h, ss=ss: e.activation(out=junk, in_=h, func=AF.Square, accum_out=ss), r=[kh, kss], w=[kss, tag + "_junk"])
            S.act(lambda e, ss=ss, rs=rs: e.activation(out=rs, in_=ss, func=AF.Ln, scale=1.0 / width, bias=self.epsb[:, 0:1]),
                  r=[kss, "epsb"], w=[kss + "r"])
            S.act(lambda e, rs=rs: e.activation(out=rs, in_=rs, func=AF.Exp, scale=-0.5), r=[kss + "r"], w=[kss + "r"])
            S.dve(lambda e, h=h, xn=xn, rs=rs: e.tensor_scalar(out=xn, in0=h, scalar1=rs, scalar2=None, op0=ALU.mult),
                  r=[kh, kss + "r"], w=[kx])
            if router is not None:
                router(c, h, kh, rs, kss + "r")
            for k0 in range(0, kc, 8):
                pb = 6 + ((c + k0 // 8) % 2)
                pT = self.bank(pb, BF16).rearrange("p (a b) -> p a b", a=8)
                kp = "bank%d" % pb
                for k in range(8):
                    S.pe(lambda e, k=k, k0=k0, xn=xn, pT=pT: e.transpose(out=pT[:, k, :], in_=xn[:, (k0 + k) * 128:(k0 + k + 1) * 128],
                                                                         identity=self.ident), r=[kx, "ident"], w=[kp])
                S.dve(lambda e, k0=k0, c=c, pT=pT: e.tensor_tensor(out=uT[:, k0:k0 + 8, c * 128:(c + 1) * 128], in0=pT,
                                                                   in1=nw[:, k0:k0 + 8].unsqueeze(2).to_broadcast([128, 8, 128]),
                                                                   op=ALU.mult), r=[kp, tag + "_nw"], w=[tag + "_uT%d" % c])

    def linear_tok(self, uT, kc, nchunks, W, col_tiles, epilogue, tag, ukey, wbufs=None, banks=(0, 1), widx=None):
        S = self.S
        Wv = W.rearrange("(k p) n -> p k n", p=128) if widx is None else None
        for ti, (c0, ncol) in enumerate(col_tiles):
            self.wrot = getattr(self, "wrot", 0) + 1
            wb, kw = wbufs[self.wrot % len(wbufs)]
            if widx is None:
                S.dma(lambda e, wb=wb, c0=c0, ncol=ncol: e.dma_start(out=wb[:, :, 0:ncol], in_=Wv[:, :, c0:c0 + ncol]),
                      w=[kw], q="pool", stream=kw)
                kws = [kw] * kc
            else:
                widx_ap, widx_key = widx
                kws = ["%s_%d" % (kw, k) for k in range(kc)]
                for k in range(kc):
                    S.dma(lambda e, wb=wb, c0=c0, ncol=ncol, k=k: e.indirect_dma_start(
                        out=wb[:, k, 0:ncol], out_offset=None, in_=W,
                        in_offset=bass.IndirectOffsetOnAxis(ap=widx_ap[:, k:k + 1], axis=0)),
                        r=[widx_key, "cv_d"], w=[kws[k]], q="pool", stream=kws[k])
            for c in range(nchunks):
                bi = banks[(ti * nchunks + c) % len(banks)]
                ps = self.bank(bi, F32, ncol)
                kp = "bank%d" % bi
                for k in range(kc):
                    S.pe(lambda e, k=k, c=c, wb=wb, ps=ps, ncol=ncol: e.matmul(ps, lhsT=uT[:, k, c * 128:(c + 1) * 128], rhs=wb[:, k, 0:ncol],
                                                                              start=(k == 0), stop=(k == kc - 1)),
                         r=[kws[k], ukey(c)], w=[kp])
                epilogue(ti, c, ps, kp)


def build_program(stop_after=None, debug=False):
    B = Builder(stop_after)
    B.debug = debug
    nc, S = B.nc, B.S
    x = B.dram_in("x", [TOK, D])
    p_in = B.dram_in("p", [2, TOK, 256])
    ssm_norm_w = B.dram_in("ssm_norm_w", [128, 8])
    ssm_w_in = B.dram_in("ssm_w_in", [D, DINP])
    conv_w = B.dram_in("conv_w", [4, 128, 3072])
    conv_b = B.dram_in("conv_b", [128, 3072])
    dt_bias = B.dram_in("dt_bias", [128, NH])
    a_log = B.dram_in("a_log", [128, NH])
    d_skip = B.dram_in("d_skip", [128, NH])
    gn_w = B.dram_in("gn_w", [128, 16])
    ssm_w_out = B.dram_in("ssm_w_out", [DI, D])
    out = nc.dram_tensor("out", [TOK, D], F32, kind="ExternalOutput").ap()
    sz_scr = B.dram_scr("sz_scr", [TOK, DI], F32)
    x_scr = B.dram_scr("x_scr", [TOK, DI], BF16)
    b_scr = B.dram_scr("b_scr", [TOK, 512], BF16)
    c_scr = B.dram_scr("c_scr", [TOK, 512], BF16)
    dt_scr = B.dram_scr("dt_scr", [TOK, NH], F32)

    B.consts()

    def stage_m1(s):
        B.reset_arena(B.const_end)
        t0 = s * L
        nw = B.sb([128, 8], F32)
        S.dma(lambda e: e.dma_start(out=nw, in_=ssm_norm_w), w=["m1_nw"], stream="m1_nw")
        uT = B.sb([128, 8, L], BF16)
        B.norm_T(x[t0:t0 + L, :], NCH, nw, uT, "m1")
        cwb = [B.sb([128, 512], BF16) for _ in range(4)]
        cbb = B.sb([128, 512], F32)
        dtb = B.sb([128, NH], F32)
        S.dma(lambda e: e.dma_start(out=dtb, in_=dt_bias), w=["dtb"], stream="dtb")
        xw = [[B.sb([128, 512], BF16) for _ in range(4)] for _ in range(2)]
        shm = []
        for j in range(1, 4):
            cur = B.sb([128, 128], BF16)
            prv = B.sb([128, 128], BF16)
            tmpf = B.sb([128, 128], F32)
            kk = "shm%d" % j
            S.pool(lambda e, tmpf=tmpf: e.memset(tmpf, 1.0), w=[kk + "t"])
            S.pool(lambda e, tmpf=tmpf, j=j: e.affine_select(out=tmpf, in_=tmpf, pattern=[[1, 128]], compare_op=ALU.is_equal,
                                                            fill=0.0, base=-j, channel_multiplier=-1), r=[kk + "t"], w=[kk + "t"])
            S.pool(lambda e, tmpf=tmpf, cur=cur: e.tensor_copy(out=cur, in_=tmpf), r=[kk + "t"], w=[kk + "c"])
            S.pool(lambda e, tmpf=tmpf: e.memset(tmpf, 1.0), r=[kk + "c"], w=[kk + "t"])
            S.pool(lambda e, tmpf=tmpf, j=j: e.affine_select(out=tmpf, in_=tmpf, pattern=[[1, 128]], compare_op=ALU.is_equal,
                                                            fill=0.0, base=128 - j, channel_multiplier=-1), r=[kk + "t"], w=[kk + "t"])
            S.pool(lambda e, tmpf=tmpf, prv=prv: e.tensor_copy(out=prv, in_=tmpf), r=[kk + "t"], w=[kk + "p"])
            shm.append((cur, prv, kk))
        stage = [B.sb([128, 512], F32) for _ in range(2)]
        stageb = [B.sb([128, 512], BF16) for _ in range(2)]
        wbufs = [(B.sb([128, 8, 512], BF16), "m1_wbuf%d" % i) for i in range(2)]
        ukey = lambda c: "m1_uT%d" % c

        def ep_z(ti, c, ps, kp):
            st = stage[c % 2]
            ks = "m1_stage%d" % (c % 2)
            S.act(lambda e: e.activation(out=st, in_=ps, func=AF.Silu), r=[kp], w=[ks])
            S.dma(lambda e: e.dma_start(out=sz_scr[t0 + c * 128:t0 + (c + 1) * 128, ti * 512:(ti + 1) * 512], in_=st),
                  r=[ks], w=["sz_scr"], stream=ks)
        B.linear_tok(uT, 8, NCH, ssm_w_in[:, 0:DI], [(i * 512, 512) for i in range(4)], ep_z, "m1z", ukey, wbufs)

        for ti in range(6):
            c0 = DI + ti * 512
            for k in range(4):
                S.dma(lambda e, k=k, ti=ti: e.dma_start(out=cwb[k], in_=conv_w[k, :, ti * 512:(ti + 1) * 512]),
                      w=["cwb%d" % k], q="pool", stream="cwb%d" % k)
            S.dma(lambda e, ti=ti: e.dma_start(out=cbb, in_=conv_b[:, ti * 512:(ti + 1) * 512]), w=["cbb"], stream="cbb")
            if ti < 4:
                dst, dc0 = x_scr, ti * 512
            elif ti == 4:
                dst, dc0 = b_scr, 0
            else:
                dst, dc0 = c_scr, 0

            def ep_conv(_ti, c, ps, kp, dst=dst, dc0=dc0):
                par = c % 2
                for k in range(4):
                    S.dve(lambda e, k=k: e.tensor_tensor(out=xw[par][k], in0=ps, in1=cwb[k], op=ALU.mult),
                          r=[kp, "cwb%d" % k], w=["xw%d_%d" % (par, k)])
                cps = B.bank(2 + par)
                kc2 = "bank%d" % (2 + par)
                mm = []
                mm.append((B.ident, "ident", xw[par][3], "xw%d_3" % par))
                for j in range(1, 4):
                    cur, prv, kk = shm[j - 1]
                    mm.append((cur, kk + "c", xw[par][3 - j], "xw%d_%d" % (par, 3 - j)))
                    if c > 0:
                        mm.append((prv, kk + "p", xw[1 - par][3 - j], "xw%d_%d" % (1 - par, 3 - j)))
                for i, (lh, lk, rh, rk) in enumerate(mm):
                    S.pe(lambda e, lh=lh, rh=rh, i=i: e.matmul(cps, lhsT=lh, rhs=rh, start=(i == 0), stop=(i == len(mm) - 1)),
                         r=[lk, rk], w=[kc2])
                st = stage[par]
                ks = "m1_stage%d" % par
                S.dve(lambda e: e.tensor_tensor(out=st, in0=cps, in1=cbb, op=ALU.add), r=[kc2, "cbb"], w=[ks])
                sb_ = stageb[par]
                ksb = "m1_stageb%d" % par
                S.act(lambda e: e.activation(out=sb_, in_=st, func=AF.Silu), r=[ks], w=[ksb])
                S.dma(lambda e: e.dma_start(out=dst[t0 + c * 128:t0 + (c + 1) * 128, dc0:dc0 + 512], in_=sb_),
                      r=[ksb], w=["xbc_scr"], stream=ksb)
            B.linear_tok(uT, 8, NCH, ssm_w_in[:, c0:c0 + 512], [(0, 512)], ep_conv, "m1x%d" % (ti % 2), ukey, wbufs)

        dts = B.sb([128, 2 * NH], F32)

        def ep_dt(ti, c, ps, kp):
            d_ = dts[:, (c % 2) * NH:(c % 2 + 1) * NH]
            kd = "m1_dts%d" % (c % 2)
            S.dve(lambda e: e.tensor_tensor(out=d_, in0=ps, in1=dtb, op=ALU.add), r=[kp, "dtb"], w=[kd])
            S.act(lambda e: e.activation(out=d_, in_=d_, func=AF.Exp), r=[kd], w=[kd])
            S.act(lambda e: e.activation(out=d_, in_=d_, func=AF.Ln, bias=B.ones[:, 0:1]), r=[kd, "ones"], w=[kd])
            S.dma(lambda e: e.dma_start(out=dt_scr[t0 + c * 128:t0 + (c + 1) * 128, :], in_=d_), r=[kd], w=["dt_scr"], stream=kd)
        B.linear_tok(uT, 8, NCH, ssm_w_in[:, DI + 3072:DINP], [(0, NH)], ep_dt, "m1d", ukey, wbufs)

    for s in range(NSEQ):
        stage_m1(s)

    if stop_after == "m1":
        S.finish()
        return B

    def stage_m2(s):
        B.reset_arena(B.const_end)
        t0 = s * L
        wout = B.sb([128, 16, D], BF16)
        S.dma(lambda e: e.dma_start(out=wout, in_=ssm_w_out.rearrange("(k p) n -> p k n", p=128)), w=["m2_wout"], q="pool", stream="m2_wout")
        Abc = B.sb([128, NH], F32)
        Dsk = B.sb([128, NH], F32)
        gnw = B.sb([128, 16], F32)
        S.dma(lambda e: e.dma_start(out=Abc, in_=a_log), w=["m2_A"], stream="m2_A")
        S.dma(lambda e: e.dma_start(out=Dsk, in_=d_skip), w=["m2_D"], stream="m2_D")
        S.dma(lambda e: e.dma_start(out=gnw, in_=gn_w), w=["m2_gnw"], stream="m2_gnw")
        S.act(lambda e: e.activation(out=Abc, in_=Abc, func=AF.Exp), r=["m2_A"], w=["m2_A"])
        S.dve(lambda e: e.tensor_scalar(out=Abc, in0=Abc, scalar1=-1.0, scalar2=None, op0=ALU.mult), r=["m2_A"], w=["m2_A"])
        state = B.sb([128, 4, 512], F32)
        stateb = B.sb([128, 4, 512], BF16)
        S.pool(lambda e: e.memset(state, 0.0), w=["st%d" % g for g in range(4)])
        S.pool(lambda e: e.memset(stateb, 0.0), w=["stb%d" % g for g in range(4)])
        xt = [B.sb([128, DI], BF16) for _ in range(2)]
        bt = [B.sb([128, 512], BF16) for _ in range(2)]
        ct = [B.sb([128, 512], BF16) for _ in range(2)]
        dtt = [B.sb([128, NH], F32) for _ in range(2)]
        szt = [B.sb([128, DI], F32) for _ in range(2)]
        ht = [B.sb([128, D], F32) for _ in range(2)]
        BT_2 = [B.sb([128, 4, 128], BF16) for _ in range(2)]
        CT_2 = [B.sb([128, 4, 128], BF16) for _ in range(2)]
        av_2 = [B.sb([128, NH], F32) for _ in range(2)]
        ex_2 = [B.sb([128, 96], F32) for _ in range(2)]
        xdt_2 = [B.sb([128, DI], BF16) for _ in range(2)]
        xds_2 = [B.sb([128, DI], BF16) for _ in range(2)]
        lh = [[B.sb([128, 128], F32) for _ in range(4)] for _ in range(2)]
        LT = [B.sb([128, 4, 128], BF16) for _ in range(2)]
        MT = [B.sb([128, 4, 128], BF16) for _ in range(2)]
        scT_2 = [B.sb([128, 4, 128], BF16) for _ in range(2)]
        yv_2 = [B.sb([128, DI], F32) for _ in range(2)]
        tmp1_2 = [B.sb([128, 512], F32) for _ in range(2)]
        tmp2_2 = [B.sb([128, 512], F32) for _ in range(2)]
        junk = B.sb([128, DI], F32)
        ynb_2 = [B.sb([128, DI], BF16) for _ in range(2)]
        ynT_2 = [B.sb([128, 16, 128], BF16) for _ in range(2)]
        stt_2 = [B.sb([128, 8], F32) for _ in range(2)]
        hout = [B.sb([128, D], F32) for _ in range(2)]

        def bc(ap, n):
            return ap.unsqueeze(2).to_broadcast([128, ap.shape[1], n])

        def v3(ap, a):
            return ap.rearrange("p (a b) -> p a b", a=a)

        def chunk(c):
            b = c % 2
            r0 = t0 + c * 128
            x_, b_, c_, d_, z_, h_ = xt[b], bt[b], ct[b], dtt[b], szt[b], ht[b]
            BT, CT, av, ex, xdt, xds, scT, yv, tmp1, tmp2, ynb, ynT, stt = [v[b] for v in (BT_2, CT_2, av_2, ex_2, xdt_2, xds_2, scT_2, yv_2, tmp1_2, tmp2_2, ynb_2, ynT_2, stt_2)]
            P_ = "p%d" % b
            kx, kb, kc_, kd, kz, kh = ["m2_%s%d" % (n, b) for n in ("x", "b", "c", "d", "z", "h")]
            S.dma(lambda e, x_=x_, r0=r0: e.dma_start(out=x_, in_=x_scr[r0:r0 + 128, :]), r=["xbc_scr"], w=[kx], stream=kx)
            S.dma(lambda e, b_=b_, r0=r0: e.dma_start(out=b_, in_=b_scr[r0:r0 + 128, :]), r=["xbc_scr"], w=[kb], stream=kb)
            S.dma(lambda e, c_=c_, r0=r0: e.dma_start(out=c_, in_=c_scr[r0:r0 + 128, :]), r=["xbc_scr"], w=[kc_], stream=kc_)
            S.dma(lambda e, d_=d_, r0=r0: e.dma_start(out=d_, in_=dt_scr[r0:r0 + 128, :]), r=["dt_scr"], w=[kd], stream=kd)
            S.dma(lambda e, z_=z_, r0=r0: e.dma_start(out=z_, in_=sz_scr[r0:r0 + 128, :]), r=["sz_scr"], w=[kz], stream=kz)
            S.dma(lambda e, h_=h_, r0=r0: e.dma_start(out=h_, in_=x[r0:r0 + 128, :]), w=[kh], stream=kh)
            pT = B.bank(7, BF16).rearrange("p (a b) -> p a b", a=8)
            for g in range(4):
                S.pe(lambda e, g=g, b_=b_: e.transpose(out=pT[:, g, :], in_=b_[:, g * 128:(g + 1) * 128], identity=B.ident), r=[kb, "ident"], w=["bank7"])
                S.pe(lambda e, g=g, c_=c_: e.transpose(out=pT[:, 4 + g, :], in_=c_[:, g * 128:(g + 1) * 128], identity=B.ident), r=[kc_, "ident"], w=["bank7"])
            S.act(lambda e: e.activation(out=BT, in_=pT[:, 0:4, :], func=AF.Identity), r=["bank7"], w=["m2_BT" + P_])
            S.act(lambda e: e.activation(out=CT, in_=pT[:, 4:8, :], func=AF.Identity), r=["bank7"], w=["m2_CT" + P_])
            S.dve(lambda e, d_=d_: e.tensor_tensor(out=av, in0=d_, in1=Abc, op=ALU.mult), r=[kd, "m2_A"], w=["m2_a" + P_])
            p0 = B.bank(0)
            S.pe(lambda e: e.matmul(p0[:, 0:32], lhsT=B.tri, rhs=av, start=True, stop=True), r=["tri", "m2_a" + P_], w=["bank0"])
            S.pe(lambda e: e.matmul(p0[:, 32:64], lhsT=B.upp, rhs=av, start=True, stop=True), r=["upp", "m2_a" + P_], w=["bank0"])
            S.pe(lambda e: e.matmul(p0[:, 64:96], lhsT=B.ones, rhs=av, start=True, stop=True), r=["ones", "m2_a" + P_], w=["bank0"])
            S.act(lambda e: e.activation(out=ex, in_=p0[:, 0:96], func=AF.Exp), r=["bank0"], w=["m2_ex" + P_])
            eacs, eacr, cd = ex[:, 0:32], ex[:, 32:64], ex[:, 64:96]
            S.dve(lambda e, x_=x_, d_=d_: e.tensor_tensor(out=v3(xdt, NH), in0=v3(x_, NH), in1=bc(d_, 64), op=ALU.mult), r=[kx, kd], w=["m2_xdt" + P_])
            S.dve(lambda e: e.tensor_tensor(out=v3(xds, NH), in0=v3(xdt, NH), in1=bc(eacr, 64), op=ALU.mult), r=["m2_xdt" + P_, "m2_ex" + P_], w=["m2_xds" + P_])
            p1 = B.bank(1)
            for g in range(4):
                S.pe(lambda e, g=g: e.matmul(p1[:, g * 128:(g + 1) * 128], lhsT=BT[:, g, :], rhs=CT[:, g, :], start=True, stop=True),
                     r=["m2_BT" + P_, "m2_CT" + P_], w=["bank1"])
            S.dve(lambda e: e.tensor_tensor(out=scT, in0=v3(p1, 4), in1=B.tri.unsqueeze(1).to_broadcast([128, 4, 128]), op=ALU.mult),
                  r=["bank1", "tri"], w=["m2_scT" + P_])
            for g in range(4):
                pyd = B.bank(4)
                for hg2 in range(2):
                    hg = g * 2 + hg2
                    pp = hg % 2
                    pseg = B.bank(2 + pp)
                    kseg = "bank%d" % (2 + pp)
                    for j in range(4):
                        hh = hg * 4 + j
                        S.dve(lambda e, j=j, hh=hh, pp=pp: e.tensor_scalar(out=lh[pp][j], in0=B.upp, scalar1=av[:, hh:hh + 1], scalar2=None, op0=ALU.mult),
                               r=["upp", "m2_a" + P_], w=["m2_lh%d_%d" % (pp, j)])
                        S.pe(lambda e, j=j, pp=pp, pseg=pseg: e.matmul(pseg[:, j * 128:(j + 1) * 128], lhsT=lh[pp][j], rhs=B.tri, start=True, stop=True),
                             r=["m2_lh%d_%d" % (pp, j), "tri"], w=[kseg])
                    S.act(lambda e, pp=pp, pseg=pseg: e.activation(out=LT[pp], in_=v3(pseg, 4), func=AF.Exp), r=[kseg], w=["m2_LT%d" % pp])
                    S.dve(lambda e, pp=pp, g=g: e.tensor_tensor(out=MT[pp], in0=LT[pp], in1=scT[:, g, :].unsqueeze(1).to_broadcast([128, 4, 128]), op=ALU.mult),
                          r=["m2_LT%d" % pp, "m2_scT" + P_], w=["m2_MT%d" % pp])
                    for j in range(4):
                        hh = hg * 4 + j
                        S.pe(lambda e, j=j, hh=hh, pp=pp, pyd=pyd: e.matmul(pyd[:, (hh % 8) * 64:(hh % 8 + 1) * 64], lhsT=MT[pp][:, j, :], rhs=xdt[:, hh * 64:(hh + 1) * 64],
                                                                  start=True, stop=True), r=["m2_MT%d" % pp, "m2_xdt" + P_], w=["bank4"])
                pyo = B.bank(5)
                pst = B.bank(6)
                S.pe(lambda e, g=g: e.matmul(pyo, lhsT=CT[:, g, :], rhs=stateb[:, g, :], start=True, stop=True), r=["m2_CT" + P_, "stb%d" % g], w=["bank5"])
                S.pe(lambda e, g=g, b_=b_: e.matmul(pst, lhsT=b_[:, g * 128:(g + 1) * 128], rhs=xds[:, g * 512:(g + 1) * 512], start=True, stop=True),
                     r=[kb, "m2_xds" + P_], w=["bank6"])
                S.dve(lambda e, g=g: e.tensor_tensor(out=v3(tmp1, 8), in0=v3(pyo, 8), in1=bc(eacs[:, g * 8:(g + 1) * 8], 64), op=ALU.mult),
                      r=["bank5", "m2_ex" + P_], w=["m2_tmp1" + P_])
                S.dve(lambda e, g=g: e.tensor_tensor(out=yv[:, g * 512:(g + 1) * 512], in0=tmp1, in1=pyd, op=ALU.add), r=["m2_tmp1" + P_, "bank4"], w=["m2_y%d" % g + P_])
                S.pool(lambda e, g=g, x_=x_: e.tensor_tensor(out=v3(tmp2, 8), in0=v3(x_[:, g * 512:(g + 1) * 512], 8), in1=bc(Dsk[:, g * 8:(g + 1) * 8], 64), op=ALU.mult),
                       r=[kx, "m2_D"], w=["m2_tmp2" + P_])
                S.dve(lambda e, g=g: e.tensor_tensor(out=yv[:, g * 512:(g + 1) * 512], in0=yv[:, g * 512:(g + 1) * 512], in1=tmp2, op=ALU.add),
                      r=["m2_tmp2" + P_, "m2_y%d" % g + P_], w=["m2_y%d" % g + P_])
                S.dve(lambda e, g=g: e.tensor_tensor(out=v3(state[:, g, :], 8), in0=v3(state[:, g, :], 8), in1=bc(cd[:, g * 8:(g + 1) * 8], 64), op=ALU.mult),
                      r=["st%d" % g, "m2_ex" + P_], w=["st%d" % g])
                S.dve(lambda e, g=g: e.tensor_tensor(out=state[:, g, :], in0=state[:, g, :], in1=pst, op=ALU.add), r=["st%d" % g, "bank6"], w=["st%d" % g])
                S.act(lambda e, g=g: e.activation(out=stateb[:, g, :], in_=state[:, g, :], func=AF.Identity), r=["st%d" % g], w=["stb%d" % g])
            S.dve(lambda e, z_=z_: e.tensor_tensor(out=yv, in0=yv, in1=z_, op=ALU.mult), r=[kz] + ["m2_y%d" % g + P_ for g in range(4)], w=["m2_yz" + P_])
            ss, rs = stt[:, 0:1], stt[:, 1:2]
            S.pool(lambda e: e.memset(ss, 0.0), w=["m2_ss" + P_])
            S.act(lambda e: e.activation(out=junk, in_=yv, func=AF.Square, accum_out=ss), r=["m2_yz" + P_, "m2_ss" + P_], w=["m2_ss" + P_, "m2_junk"])
            S.act(lambda e: e.activation(out=rs, in_=ss, func=AF.Ln, scale=1.0 / DI, bias=B.epsb[:, 0:1]), r=["m2_ss" + P_, "epsb"], w=["m2_rs" + P_])
            S.act(lambda e: e.activation(out=rs, in_=rs, func=AF.Exp, scale=-0.5), r=["m2_rs" + P_], w=["m2_rs" + P_])
            S.dve(lambda e: e.tensor_scalar(out=ynb, in0=yv, scalar1=rs, scalar2=None, op0=ALU.mult), r=["m2_yz" + P_, "m2_rs" + P_], w=["m2_ynb" + P_])
            for k0 in (0, 8):
                pb = 6 + (k0 // 8)
                pTn = B.bank(pb, BF16).rearrange("p (a b) -> p a b", a=8)
                for k in range(8):
                    S.pe(lambda e, k=k, k0=k0, pTn=pTn: e.transpose(out=pTn[:, k, :], in_=ynb[:, (k0 + k) * 128:(k0 + k + 1) * 128], identity=B.ident),
                         r=["m2_ynb" + P_, "ident"], w=["bank%d" % pb])
                S.dve(lambda e, k0=k0, pTn=pTn: e.tensor_tensor(out=ynT[:, k0:k0 + 8, :], in0=pTn, in1=bc(gnw[:, k0:k0 + 8], 128), op=ALU.mult),
                      r=["bank%d" % pb, "m2_gnw"], w=["m2_ynT%d" % k0 + P_])
            ho = hout[b]
            for half in range(2):
                po = B.bank(half)
                for k in range(16):
                    S.pe(lambda e, k=k, half=half, po=po: e.matmul(po, lhsT=ynT[:, k, :], rhs=wout[:, k, half * 512:(half + 1) * 512], start=(k == 0), stop=(k == 15)),
                         r=["m2_ynT0" + P_, "m2_ynT8" + P_, "m2_wout"], w=["bank%d" % half])
                S.dve(lambda e, half=half, po=po, ho=ho, h_=h_: e.tensor_tensor(out=ho[:, half * 512:(half + 1) * 512], in0=h_[:, half * 512:(half + 1) * 512], in1=po, op=ALU.add),
                      r=["bank%d" % half, kh], w=["m2_ho%d" % b])
            S.dma(lambda e, ho=ho, r0=r0: e.dma_start(out=out[r0:r0 + 128, :], in_=ho), r=["m2_ho%d" % b], w=["out"], stream="m2_ho%d" % b)

        for c in range(NCH):
            chunk(c)

    for s in range(NSEQ):
        stage_m2(s)

    if stop_after == "m2":
        S.finish()
        return B

    TT = 1024
    NTC = TT // 128

    def stage_ffn(tile, tag, norm_w_d, wg_list, wu_list, wd_list, w_router=None):
        B.reset_arena(B.const_end)
        r0 = tile * TT
        ne = len(wg_list)
        nw = B.sb([128, 8], F32)
        S.dma(lambda e: e.dma_start(out=nw, in_=norm_w_d), w=[tag + "_nw"], stream=tag + "_nw")
        uT = B.sb([128, 8, TT], BF16)
        actT = B.sb([128, 28, TT], BF16)
        acc = B.sb([128, NTC, D], F32)
        gates = B.sb([128, NTC, 8], F32)
        wb8 = [(B.sb([128, 8, 512], BF16), tag + "_wb8_%d" % i) for i in range(2)]
        wb28 = [(B.sb([128, 28, 256], BF16), tag + "_wb28_%d" % i) for i in range(2)]
        S.dma(lambda e: e.dma_start(out=acc, in_=out[r0:r0 + TT, :].rearrange("(c p) n -> p c n", p=128)), r=["out"], w=[tag + "_acc%d" % c for c in range(NTC)],
              stream=tag + "_acc")
        mark = B.aoff
        router = None
        if w_router is not None:
            wr = B.sb([128, 8, 8], F32)
            S.dma(lambda e: e.dma_start(out=wr, in_=w_router.rearrange("(k p) n -> p k n", p=128)), w=[tag + "_wr"], stream=tag + "_wr")
            xn32 = B.sb([128, D], F32)
            u32T = B.sb([128, 8, 128], F32)
            lg = B.sb([128, 8], F32)
            lg2 = B.sb([128, 8], F32)
            mk1 = B.sb([128, 8], F32)
            mk2 = B.sb([128, 8], F32)
            sm = B.sb([128, 8], F32)

            def router(c, h, kh, rs, krs):
                S.dve(lambda e: e.tensor_scalar(out=xn32, in0=h, scalar1=rs, scalar2=None, op0=ALU.mult), r=[kh, krs], w=[tag + "_xn32"])
                for half in range(2):
                    pb = 4 + half
                    pT = B.bank(pb).rearrange("p (a b) -> p a b", a=4)
                    for k in range(4):
                        kk = half * 4 + k
                        S.pe(lambda e, k=k, kk=kk, pT=pT: e.transpose(out=pT[:, k, :], in_=xn32[:, kk * 128:(kk + 1) * 128], identity=B.identf),
                             r=[tag + "_xn32", "identf"], w=["bank%d" % pb])
                    S.dve(lambda e, half=half, pT=pT: e.tensor_tensor(out=u32T[:, half * 4:(half + 1) * 4, :], in0=pT,
                                                                      in1=nw[:, half * 4:(half + 1) * 4].unsqueeze(2).to_broadcast([128, 4, 128]), op=ALU.mult),
                          r=["bank%d" % pb, tag + "_nw"], w=[tag + "_u32T%d" % half])
                pl = B.bank(3, F32, 8)
                for k in range(8):
                    S.pe(lambda e, k=k: e.matmul(pl, lhsT=u32T[:, k, :], rhs=wr[:, k, :], start=(k == 0), stop=(k == 7)),
                         r=[tag + "_u32T0", tag + "_u32T1", tag + "_wr"], w=["bank3"])
                m1, m2, dm, w1, w2 = sm[:, 0:1], sm[:, 1:2], sm[:, 2:3], sm[:, 3:4], sm[:, 4:5]
                kq = tag + "_rt"
                S.dve(lambda e: e.tensor_copy(out=lg, in_=pl), r=["bank3"], w=[kq + "lg"])
                S.dve(lambda e: e.reduce_max(out=m1, in_=lg, axis=AX.X), r=[kq + "lg"], w=[kq + "m1"])
                S.dve(lambda e: e.tensor_scalar(out=mk1, in0=lg, scalar1=m1, scalar2=None, op0=ALU.is_equal), r=[kq + "lg", kq + "m1"], w=[kq + "mk1"])
                S.dve(lambda e: e.scalar_tensor_tensor(out=lg2, in0=mk1, scalar=-1e30, in1=lg, op0=ALU.mult, op1=ALU.add), r=[kq + "mk1", kq + "lg"], w=[kq + "lg2"])
                S.dve(lambda e: e.reduce_max(out=m2, in_=lg2, axis=AX.X), r=[kq + "lg2"], w=[kq + "m2"])
                S.dve(lambda e: e.tensor_scalar(out=mk2, in0=lg2, scalar1=m2, scalar2=None, op0=ALU.is_equal), r=[kq + "lg2", kq + "m2"], w=[kq + "mk2"])
                S.dve(lambda e: e.tensor_tensor(out=dm, in0=m2, in1=m1, op=ALU.subtract), r=[kq + "m1", kq + "m2"], w=[kq + "dm"])
                S.act(lambda e: e.activation(out=dm, in_=dm, func=AF.Exp), r=[kq + "dm"], w=[kq + "dm"])
                S.dve(lambda e: e.tensor_scalar(out=w1, in0=dm, scalar1=1.0, scalar2=None, op0=ALU.add), r=[kq + "dm"], w=[kq + "w1"])
                S.dve(lambda e: e.reciprocal(out=w1, in_=w1), r=[kq + "w1"], w=[kq + "w1"])
                S.dve(lambda e: e.tensor_tensor(out=w2, in0=dm, in1=w1, op=ALU.mult), r=[kq + "dm", kq + "w1"], w=[kq + "w2"])
                S.dve(lambda e: e.tensor_scalar(out=mk1, in0=mk1, scalar1=w1, scalar2=None, op0=ALU.mult), r=[kq + "mk1", kq + "w1"], w=[kq + "mk1"])
                S.dve(lambda e, c=c: e.scalar_tensor_tensor(out=gates[:, c, :], in0=mk2, scalar=w2, in1=mk1, op0=ALU.mult, op1=ALU.add),
                      r=[kq + "mk2", kq + "w2", kq + "mk1"], w=[tag + "_gates%d" % c])
        else:
            S.pool(lambda e: e.memset(gates, 1.0), w=[tag + "_gates%d" % c for c in range(NTC)])
        B.norm_T(out[r0:r0 + TT, :], NTC, nw, uT, tag, router=router)
        B.reset_arena(mark)
        sg = B.sb([128, NTC, 512], F32)
        ab = [B.sb([128, 512], BF16) for _ in range(2)]
        ukey = lambda c: tag + "_uT%d" % c
        akey = lambda c: tag + "_actT%d" % c
        for ei in range(ne):
            for ft in range(7):
                def ep_gate(ti, c, ps, kp):
                    S.act(lambda e: e.activation(out=sg[:, c, :], in_=ps, func=AF.Silu), r=[kp], w=[tag + "_sg%d" % c])

                def ep_up(ti, c, ps, kp, ft=ft):
                    a_ = ab[c % 2]
                    ka = tag + "_ab%d" % (c % 2)
                    S.dve(lambda e: e.tensor_tensor(out=a_, in0=sg[:, c, :], in1=ps, op=ALU.mult), r=[kp, tag + "_sg%d" % c], w=[ka])
                    pb = 6 + (c % 2)
                    pT = B.bank(pb, BF16)[:, 0:512].rearrange("p (a b) -> p a b", a=4)
                    for j in range(4):
                        S.pe(lambda e, j=j: e.transpose(out=pT[:, j, :], in_=a_[:, j * 128:(j + 1) * 128], identity=B.ident), r=[ka, "ident"], w=["bank%d" % pb])
                    S.act(lambda e: e.activation(out=actT[:, ft * 4:(ft + 1) * 4, c * 128:(c + 1) * 128], in_=pT, func=AF.Identity), r=["bank%d" % pb], w=[akey(c)])
                B.linear_tok(uT, 8, NTC, wg_list[ei][:, ft * 512:(ft + 1) * 512], [(0, 512)], ep_gate, tag + "g", ukey, wb8, banks=(0, 1))
                B.linear_tok(uT, 8, NTC, wu_list[ei][:, ft * 512:(ft + 1) * 512], [(0, 512)], ep_up, tag + "u", ukey, wb8, banks=(2, 3))

            def ep_down(ti, c, ps, kp, ei=ei):
                S.dve(lambda e: e.scalar_tensor_tensor(out=acc[:, c, ti * 256:(ti + 1) * 256], in0=ps, scalar=gates[:, c, ei:ei + 1],
                                                       in1=acc[:, c, ti * 256:(ti + 1) * 256], op0=ALU.mult, op1=ALU.add),
                      r=[kp, tag + "_gates%d" % c, tag + "_acc%d" % c], w=[tag + "_acc%d" % c])
            B.linear_tok(actT, 28, NTC, wd_list[ei], [(i * 256, 256) for i in range(4)], ep_down, tag + "d", akey, wb28, banks=(4, 5))
        S.dma(lambda e: e.dma_start(out=out[r0:r0 + TT, :].rearrange("(c p) n -> p c n", p=128), in_=acc), r=[tag + "_acc%d" % c for c in range(NTC)], w=["out"],
              stream=tag + "_acc")

    ffn_norm_w = B.dram_in("ffn_norm_w", [128, 8])
    ffn_w_gate = B.dram_in("ffn_w_gate", [D, DFF])
    ffn_w_up = B.dram_in("ffn_w_up", [D, DFF])
    ffn_w_down = B.dram_in("ffn_w_down", [DFF, D])
    for tile in range(TOK // TT):
        stage_ffn(tile, "ff", ffn_norm_w, [ffn_w_gate], [ffn_w_up], [ffn_w_down])

    if stop_after == "ffn":
        S.finish()
        return B

    ple_norm_w = B.dram_in("ple_norm_w", [2, 128, 8])
    ple_w_gate = B.dram_in("ple_w_gate", [2, D, D])
    ple_w_proj = B.dram_in("ple_w_proj", [2, 256, D])

    def stage_ple(tile, li):
        tag = "pl"
        B.reset_arena(B.const_end)
        r0 = tile * TT
        nw = B.sb([128, 8], F32)
        S.dma(lambda e: e.dma_start(out=nw, in_=ple_norm_w[li]), w=[tag + "_nw"], stream=tag + "_nw")
        uT = B.sb([128, 8, TT], BF16)
        ppT = B.sb([128, 2, TT], BF16)
        acc = B.sb([128, NTC, D], F32)
        sgm = B.sb([128, NTC, 512], F32)
        wb8 = [(B.sb([128, 8, 512], BF16), tag + "_wb8_%d" % i) for i in range(2)]
        wb2 = [(B.sb([128, 2, 512], BF16), tag + "_wb2_%d" % i) for i in range(2)]
        pld = [B.sb([128, 256], F32) for _ in range(2)]
        plb = [B.sb([128, 256], BF16) for _ in range(2)]
        dl = [B.sb([128, 512], F32) for _ in range(2)]
        S.dma(lambda e: e.dma_start(out=acc, in_=out[r0:r0 + TT, :].rearrange("(c p) n -> p c n", p=128)), r=["out"], w=[tag + "_acc%d" % c for c in range(NTC)],
              stream=tag + "_acc")
        B.norm_T(out[r0:r0 + TT, :], NTC, nw, uT, tag)
        for c in range(NTC):
            b = c % 2
            kl = tag + "_pld%d" % b
            S.dma(lambda e, c=c, b=b: e.dma_start(out=pld[b], in_=p_in[li, r0 + c * 128:r0 + (c + 1) * 128, :]), w=[kl], stream=kl)
            S.dve(lambda e, b=b: e.tensor_copy(out=plb[b], in_=pld[b]), r=[kl], w=[kl + "b"])
            pT = B.bank(4 + b, BF16)[:, 0:256].rearrange("p (a b) -> p a b", a=2)
            for k in range(2):
                S.pe(lambda e, k=k, b=b, pT=pT: e.transpose(out=pT[:, k, :], in_=plb[b][:, k * 128:(k + 1) * 128], identity=B.ident), r=[kl + "b", "ident"], w=["bank%d" % (4 + b)])
            S.act(lambda e, c=c, pT=pT: e.activation(out=ppT[:, :, c * 128:(c + 1) * 128], in_=pT, func=AF.Identity), r=["bank%d" % (4 + b)], w=[tag + "_ppT%d" % c])
        ukey = lambda c: tag + "_uT%d" % c
        pkey = lambda c: tag + "_ppT%d" % c
        for ct_ in range(2):
            def ep_g(ti, c, ps, kp):
                S.act(lambda e: e.activation(out=sgm[:, c, :], in_=ps, func=AF.Sigmoid), r=[kp], w=[tag + "_sgm%d" % c])

            def ep_p(ti, c, ps, kp, ct_=ct_):
                d_ = dl[c % 2]
                kd = tag + "_dl%d" % (c % 2)
                S.dve(lambda e: e.tensor_tensor(out=d_, in0=sgm[:, c, :], in1=ps, op=ALU.mult), r=[kp, tag + "_sgm%d" % c], w=[kd])
                S.dve(lambda e: e.tensor_tensor(out=acc[:, c, ct_ * 512:(ct_ + 1) * 512], in0=acc[:, c, ct_ * 512:(ct_ + 1) * 512], in1=d_, op=ALU.add),
                      r=[kd, tag + "_acc%d" % c], w=[tag + "_acc%d" % c])
            B.linear_tok(uT, 8, NTC, ple_w_gate[li][:, ct_ * 512:(ct_ + 1) * 512], [(0, 512)], ep_g, tag + "g", ukey, wb8, banks=(0, 1))
            B.linear_tok(ppT, 2, NTC, ple_w_proj[li][:, ct_ * 512:(ct_ + 1) * 512], [(0, 512)], ep_p, tag + "p", pkey, wb2, banks=(2, 3))
        S.dma(lambda e: e.dma_start(out=out[r0:r0 + TT, :].rearrange("(c p) n -> p c n", p=128), in_=acc), r=[tag + "_acc%d" % c for c in range(NTC)], w=["out"],
              stream=tag + "_acc")

    for tile in range(TOK // TT):
        stage_ple(tile, 0)

    if stop_after == "ple0":
        S.finish()
        return B

    kv_norm_w = B.dram_in("kv_norm_w", [128, 8])
    w_kv = B.dram_in("w_kv", [D, 2064])
    b_f = B.dram_in("b_f", [128, 16])
    k_norm_w = B.dram_in("k_norm_w", [128, 64])
    att_norm_w = B.dram_in("att_norm_w", [128, 8])
    att_w_q = B.dram_in("att_w_q", [D, D])
    q_norm_w = B.dram_in("q_norm_w", [128, 64])
    att_w_o = B.dram_in("att_w_o", [D, D])

    def bc(ap, n):
        return ap.unsqueeze(2).to_broadcast([128, ap.shape[1], n])

    def v3(ap, a):
        return ap.rearrange("p (a b) -> p a b", a=a)

    moe_w_gate = B.dram_in("moe_w_gate", [NE, D, DFF])
    moe_w_up = B.dram_in("moe_w_up", [NE, D, DFF])
    moe_w_down = B.dram_in("moe_w_down", [NE, DFF, D])
    wgub = B.dram_scr("wgub", [NE * D, 7, 2, 512], BF16)
    wdb = B.dram_scr("wdb", [NE * DFF, D], BF16)
    conv_jobs = []
    for e_ in range(NE):
        conv_jobs.append((wgub[e_ * D:(e_ + 1) * D, :, 0, :], moe_w_gate[e_].rearrange("r (t n) -> r t n", n=512), "cv_g"))
        conv_jobs.append((wgub[e_ * D:(e_ + 1) * D, :, 1, :], moe_w_up[e_].rearrange("r (t n) -> r t n", n=512), "cv_u"))
        conv_jobs.append((wdb[e_ * DFF:(e_ + 1) * DFF, :], moe_w_down[e_], "cv_d"))

    def issue_conv(n):
        for _ in range(n):
            if conv_jobs:
                dst, src, key = conv_jobs.pop(0)
                S.dma(lambda e, dst=dst, src=src: e.dma_start(out=dst, in_=src), w=[key], q="pool", stream=key, nobar=True)

    def stage_attn(s):
        tag = "at"
        B.reset_arena(B.const_end)
        t0 = s * L
        KT = B.sb([128, 16, L], BF16)
        Vaug = B.sb([128, NCH * 16 * 65], BF16).rearrange("p (c h d) -> p c h d", c=NCH, h=16)
        negc = B.sb([128, NCH, 16], F32)
        c3 = B.sb([128, NCH * 48], BF16).rearrange("p (c h j) -> p c h j", c=NCH, h=16)
        nlf = B.sb([128, NCH, 16], F32)
        knw = B.sb([128, 64], F32)
        qnw = B.sb([128, 64], F32)
        bfb = B.sb([128, 16], F32)
        lnq = B.sb([128, 8], F32)
        S.dma(lambda e: e.dma_start(out=knw, in_=k_norm_w), w=["at_knw"], stream="at_knw")
        S.dma(lambda e: e.dma_start(out=qnw, in_=q_norm_w), w=["at_qnw"], stream="at_qnw")
        S.dma(lambda e: e.dma_start(out=bfb, in_=b_f), w=["at_bfb"], stream="at_bfb")
        S.pool(lambda e: e.memset(lnq, float(np.log(0.125))), w=["at_lnq"])
        S.pool(lambda e: e.memset(Vaug, 1.0), w=["at_V"])
        mark1 = B.aoff
        nwk = B.sb([128, 8], F32)
        S.dma(lambda e: e.dma_start(out=nwk, in_=kv_norm_w), w=["kv_nw"], stream="kv_nw")
        uT = B.sb([128, 8, L], BF16)
        B.norm_T(out[t0:t0 + L, :], NCH, nwk, uT, "kv")
        wb8 = [(B.sb([128, 8, 512], BF16), "kv_wb8_%d" % i) for i in range(2)]
        kaug = [B.sb([128, 8 * 67], BF16).rearrange("p (h d) -> p h d", h=8) for _ in range(2)]
        hnb = [(B.sb([128, 512], F32), B.sb([128, 512], F32), B.sb([128, 8], F32)) for _ in range(2)]
        tf = B.sb([128, 16], F32)
        carry = B.sb([128, 16], F32)
        cs32 = B.sb([128, 16], F32)
        cs32b = B.sb([128, 16], F32)
        for i in range(2):
            S.pool(lambda e, i=i: e.memset(kaug[i], 1.0), w=["kv_kaug%d" % i])
        ukey = lambda c: "kv_uT%d" % c

        def headnorm(ps, kp, nwt, nwkey, dst, dstkey, qscale, bufs, sfx):
            sq, kn32, ssk = bufs
            ksq, kss, kkn = "hn_sq" + sfx, "hn_ssk" + sfx, "hn_kn32" + sfx
            S.act(lambda e: e.activation(out=sq, in_=ps, func=AF.Square), r=[kp], w=[ksq])
            S.dve(lambda e: e.reduce_sum(out=ssk, in_=v3(sq, 8), axis=AX.X), r=[ksq], w=[kss])
            S.act(lambda e: e.activation(out=ssk, in_=ssk, func=AF.Ln, scale=1.0 / 64, bias=B.epsb[:, 0:1]), r=[kss, "epsb"], w=[kss])
            if qscale:
                S.act(lambda e: e.activation(out=ssk, in_=ssk, func=AF.Exp, scale=-0.5, bias=lnq[:, 0:1]), r=[kss, "at_lnq"], w=[kss])
            else:
                S.act(lambda e: e.activation(out=ssk, in_=ssk, func=AF.Exp, scale=-0.5), r=[kss], w=[kss])
            S.dve(lambda e: e.tensor_tensor(out=v3(kn32, 8), in0=v3(ps, 8), in1=bc(ssk, 64), op=ALU.mult), r=[kp, kss], w=[kkn])
            S.dve(lambda e: e.tensor_tensor(out=dst, in0=v3(kn32, 8), in1=nwt.unsqueeze(1).to_broadcast([128, 8, 64]), op=ALU.mult),
                  r=[kkn, nwkey], w=[dstkey])

        def ep_k(ti, c, ps, kp):
            ka = kaug[c % 2]
            kk = "kv_kaug%d" % (c % 2)
            headnorm(ps, kp, knw, "at_knw", ka[:, :, 0:64], kk, False, hnb[c % 2], "k%d" % (c % 2))
            pb = 4 + (c % 2)
            pT = B.bank(pb, BF16).rearrange("p (a b) -> p a b", a=8)
            for j in range(8):
                S.pe(lambda e, j=j: e.transpose(out=pT[0:67, j, :], in_=ka[:, j, :], identity=B.ident), r=[kk, "ident"], w=["bank%d" % pb])
            S.act(lambda e: e.activation(out=KT[0:67, ti * 8:(ti + 1) * 8, c * 128:(c + 1) * 128], in_=pT[0:67], func=AF.Identity), r=["bank%d" % pb], w=["at_KT"])
        B.linear_tok(uT, 8, NCH, w_kv[:, 0:1024], [(0, 512), (512, 512)], ep_k, "kvk", ukey, wb8, banks=(0, 1, 2, 3))

        def ep_v(ti, c, ps, kp):
            S.act(lambda e: e.activation(out=Vaug[:, c, ti * 8:(ti + 1) * 8, 0:64], in_=v3(ps, 8), func=AF.Identity), r=[kp], w=["at_V"])
        B.linear_tok(uT, 8, NCH, w_kv[:, 1024:2048], [(0, 512), (512, 512)], ep_v, "kvv", ukey, wb8, banks=(0, 1))

        def ep_f(ti, c, ps, kp):
            S.dve(lambda e: e.tensor_tensor(out=tf, in0=ps, in1=bfb, op=ALU.add), r=[kp, "at_bfb"], w=["kv_tf"])
            S.act(lambda e: e.activation(out=tf, in_=tf, func=AF.Exp, scale=-1.0), r=["kv_tf"], w=["kv_tf"])
            S.act(lambda e: e.activation(out=nlf[:, c, :], in_=tf, func=AF.Ln, bias=B.ones[:, 0:1]), r=["kv_tf", "ones"], w=["at_nlf%d" % c])
        B.linear_tok(uT, 8, NCH, w_kv[:, 2048:2064], [(0, 16)], ep_f, "kvf", ukey, wb8, banks=(0, 1))
        S.pool(lambda e: e.memset(carry, 0.0), w=["kv_carry"])
        for c in range(NCH):
            pc = B.bank(2 + (c % 2))
            kpc = "bank%d" % (2 + (c % 2))
            S.pe(lambda e, c=c, pc=pc: e.matmul(pc[:, 0:16], lhsT=B.tri, rhs=nlf[:, c, :], start=True, stop=True), r=["tri", "at_nlf%d" % c], w=[kpc])
            S.pe(lambda e, c=c, pc=pc: e.matmul(pc[:, 16:32], lhsT=B.ones, rhs=nlf[:, c, :], start=True, stop=True), r=["ones", "at_nlf%d" % c], w=[kpc])
            S.dve(lambda e, c=c, pc=pc: e.tensor_tensor(out=negc[:, c, :], in0=pc[:, 0:16], in1=carry, op=ALU.add), r=[kpc, "kv_carry"], w=["at_negc%d" % c])
            S.dve(lambda e, pc=pc: e.tensor_tensor(out=carry, in0=pc[:, 16:32], in1=carry, op=ALU.add), r=[kpc, "kv_carry"], w=["kv_carry"])
            kc3 = "at_c3_%d" % c
            S.dve(lambda e, c=c: e.tensor_scalar(out=cs32, in0=negc[:, c, :], scalar1=-1.0, scalar2=None, op0=ALU.mult), r=["at_negc%d" % c], w=["kv_cs32"])
            for j in range(3):
                S.dve(lambda e, c=c, j=j: e.tensor_copy(out=c3[:, c, :, j], in_=cs32), r=["kv_cs32"], w=[kc3])
                if j < 2:
                    S.dve(lambda e, c=c, j=j: e.tensor_copy(out=cs32b, in_=c3[:, c, :, j]), r=[kc3], w=["kv_cs32b"])
                    S.dve(lambda e: e.tensor_tensor(out=cs32, in0=cs32, in1=cs32b, op=ALU.subtract), r=["kv_cs32", "kv_cs32b"], w=["kv_cs32"])

        if s == 0:
            B.dbg("negc", negc, ["at_negc%d" % c for c in range(NCH)])
            B.dbg("nlf", nlf, ["at_nlf%d" % c for c in range(NCH)])
            B.dbg("c3", c3, ["at_c3_%d" % c for c in range(NCH)])
            B.dbg("KT", KT, ["at_KT"])
            B.dbg("Vaug", Vaug, ["at_V"])
        B.reset_arena(mark1)
        wq = B.sb([128, 8, D], BF16)
        wo = B.sb([128, 8, D], BF16)
        nwa = B.sb([128, 8], F32)
        S.dma(lambda e: e.dma_start(out=wq, in_=att_w_q.rearrange("(k p) n -> p k n", p=128)), w=["at_wq"], q="pool", stream="at_wq")
        S.dma(lambda e: e.dma_start(out=wo, in_=att_w_o.rearrange("(k p) n -> p k n", p=128)), w=["at_wo"], q="pool", stream="at_wo")
        S.dma(lambda e: e.dma_start(out=nwa, in_=att_norm_w), w=["aq_nw"], stream="aq_nw")
        mark2 = B.aoff
        def qtile(qt):
            B.reset_arena(mark2)
            issue_conv(3)
            q00 = t0 + qt * 512
            QT = B.sb([128, 16, 512], BF16)
            qaug = [B.sb([128, 16 * 67], BF16).rearrange("p (h d) -> p h d", h=16) for _ in range(2)]
            hnq = [(B.sb([128, 512], F32), B.sb([128, 512], F32), B.sb([128, 8], F32)) for _ in range(2)]
            mark3 = B.aoff
            uTq = B.sb([128, 8, 512], BF16)
            B.norm_T(out[q00:q00 + 512, :], 4, nwa, uTq, "aq")
            for m in range(4):
                qa = qaug[m % 2]
                kqa = "at_qaug%d" % (m % 2)
                for half in range(2):
                    pq = B.bank(6 + half)
                    kpq = "bank%d" % (6 + half)
                    for k in range(8):
                        S.pe(lambda e, k=k, m=m, half=half, pq=pq: e.matmul(pq, lhsT=uTq[:, k, m * 128:(m + 1) * 128], rhs=wq[:, k, half * 512:(half + 1) * 512],
                                                                            start=(k == 0), stop=(k == 7)), r=["aq_uT%d" % m, "at_wq"], w=[kpq])
                    headnorm(pq, kpq, qnw, "at_qnw", qa[:, half * 8:(half + 1) * 8, 0:64], kqa, True, hnq[half], "q%d" % half)
                S.dve(lambda e, qa=qa, m=m: e.tensor_copy(out=qa[:, :, 64:67], in_=c3[:, 4 * qt + m, :, :]), r=["at_c3_%d" % (4 * qt + m)], w=[kqa])
                for half in range(2):
                    pb = 6 + half
                    pT = B.bank(pb, BF16).rearrange("p (a b) -> p a b", a=8)
                    for j in range(8):
                        S.pe(lambda e, j=j, half=half, qa=qa, pT=pT: e.transpose(out=pT[0:67, j, :], in_=qa[:, half * 8 + j, :], identity=B.ident),
                             r=[kqa, "ident"], w=["bank%d" % pb])
                    S.act(lambda e, half=half, m=m, pT=pT: e.activation(out=QT[0:67, half * 8:(half + 1) * 8, m * 128:(m + 1) * 128], in_=pT[0:67], func=AF.Identity),
                          r=["bank%d" % pb], w=["at_QT"])
            B.reset_arena(mark3)
            PT = [B.sb([128, 512], BF16) for _ in range(2)]
            osb = B.sb([128, 4, D], BF16)
            rinv = B.sb([128, 8], F32)
            oT = B.sb([128, 8, 128], BF16)
            hq = [B.sb([128, D], F32) for _ in range(2)]
            nj = 4 * qt + 4
            steps = [(hh, j) for hh in range(16) for j in range(nj)]

            def emit_S(i):
                hh, j = steps[i]
                q0 = max(j - 4 * qt, 0) * 128
                pb = i % 2
                pst = B.bank(pb)
                pt = PT[pb]
                kpt = "at_PT%d" % pb
                S.pe(lambda e: e.matmul(pst[:, q0:512], lhsT=KT[0:67, hh, j * 128:(j + 1) * 128], rhs=QT[0:67, hh, q0:512], start=True, stop=True),
                     r=["at_KT", "at_QT"], w=["bank%d" % pb])
                S.act(lambda e: e.activation(out=pt[:, q0:512], in_=pst[:, q0:512], func=AF.Exp, bias=negc[:, j, hh:hh + 1]),
                      r=["bank%d" % pb, "at_negc%d" % j], w=[kpt])
                if j - 4 * qt >= 0:
                    S.dve(lambda e: e.tensor_tensor(out=pt[:, q0:q0 + 128], in0=pt[:, q0:q0 + 128], in1=B.trib, op=ALU.mult), r=[kpt, "trib"], w=[kpt])

            def emit_PV(i):
                hh, j = steps[i]
                m0 = max(j - 4 * qt, 0)
                pt = PT[i % 2]
                kpt = "at_PT%d" % (i % 2)
                for m in range(m0, 4):
                    po = B.bank(2 + m, F32, 65)
                    S.pe(lambda e, m=m, po=po: e.matmul(po, lhsT=pt[:, m * 128:(m + 1) * 128], rhs=Vaug[:, j, hh, :], start=(j == 0), stop=(j == 4 * qt + m)),
                         r=[kpt, "at_V"], w=["bank%d" % (2 + m)])
                if j == nj - 1:
                    for m in range(4):
                        po = B.bank(2 + m, F32, 65)
                        S.dve(lambda e, m=m, po=po: e.reciprocal(out=rinv[:, m:m + 1], in_=po[:, 64:65]), r=["bank%d" % (2 + m)], w=["at_rinv%d" % m])
                        S.dve(lambda e, m=m, po=po: e.tensor_scalar(out=osb[:, m, hh * 64:(hh + 1) * 64], in0=po[:, 0:64], scalar1=rinv[:, m:m + 1], scalar2=None, op0=ALU.mult),
                              r=["bank%d" % (2 + m), "at_rinv%d" % m], w=["at_osb%d" % m])

            emit_S(0)
            for i in range(len(steps)):
                if i + 1 < len(steps):
                    emit_S(i + 1)
                emit_PV(i)
            if s == 0 and qt == 0:
                B.dbg("QT", QT, ["at_QT"])
                B.dbg("osb", osb, ["at_osb%d" % m for m in range(4)])
                B.dbg("rinv", rinv, ["at_rinv%d" % m for m in range(4)])
                B.dbg("PT0", PT[0], ["at_PT0"])
            for m in range(4):
                r0 = q00 + m * 128
                h_ = hq[m % 2]
                khq = "at_hq%d" % (m % 2)
                S.dma(lambda e, h_=h_, r0=r0: e.dma_start(out=h_, in_=out[r0:r0 + 128, :]), r=["out"], w=[khq], stream=khq)
                pT = B.bank(6, BF16).rearrange("p (a b) -> p a b", a=8)
                for k in range(8):
                    S.pe(lambda e, k=k, m=m, pT=pT: e.transpose(out=pT[:, k, :], in_=osb[:, m, k * 128:(k + 1) * 128], identity=B.ident), r=["at_osb%d" % m, "ident"], w=["bank6"])
                S.act(lambda e, pT=pT: e.activation(out=oT, in_=pT, func=AF.Identity), r=["bank6"], w=["at_oT"])
                for half in range(2):
                    pw = B.bank(half)
                    for k in range(8):
                        S.pe(lambda e, k=k, half=half, pw=pw: e.matmul(pw, lhsT=oT[:, k, :], rhs=wo[:, k, half * 512:(half + 1) * 512], start=(k == 0), stop=(k == 7)),
                             r=["at_oT", "at_wo"], w=["bank%d" % half])
                    S.dve(lambda e, half=half, pw=pw, h_=h_: e.tensor_tensor(out=h_[:, half * 512:(half + 1) * 512], in0=h_[:, half * 512:(half + 1) * 512], in1=pw, op=ALU.add),
                          r=["bank%d" % half, khq], w=[khq])
                S.dma(lambda e, h_=h_, r0=r0: e.dma_start(out=out[r0:r0 + 128, :], in_=h_), r=[khq], w=["out"], stream=khq)

        for qt in range(4):
            qtile(qt)

    for s in range(NSEQ):
        stage_attn(s)

    if stop_after == "attn":
        S.finish()
        return B

    moe_norm_w = B.dram_in("moe_norm_w", [128, 8])
    moe_w_router = B.dram_in("moe_w_router", [D, NE])
    issue_conv(99)

    UT = 512
    UNTC = UT // 128
    NU = 24
    NSLOT = NU * UT
    u_scr = B.dram_scr("u_scr", [TOK, D], BF16)
    slot_tok = B.dram_scr("slot_tok", [NSLOT, 1], I32)
    y_slots = B.dram_scr("y_slots", [NSLOT, D], F32)
    wgu2d = wgub.rearrange("r t g n -> (r t) (g n)")
    wd2d = wdb.rearrange("r (t n) -> (r t) n", n=512)
    NC32 = TOK // 128

    B.reset_arena(B.const_end)
    slots_i = B.sb([128, 2 * NC32], I32)
    slots_f = B.sb([128, 2 * NC32], F32)
    W12 = B.sb([128, 2 * NC32], F32)
    M1 = B.sb([128, NC32, 8], F32)
    M2 = B.sb([128, NC32, 8], F32)
    POS = B.sb([128, NC32, 8], F32)
    idxg = B.sb([128, 7, NU * 8], I32)
    idxd = B.sb([128, 2, NU * 28], I32)
    nwm = B.sb([128, 8], F32)
    S.dma(lambda e: e.dma_start(out=nwm, in_=moe_norm_w), w=["mo_nw"], stream="mo_nw")
    pmark = B.aoff

    def stage_route():
        tag = "rt"
        wr = B.sb([128, 8, 8], F32)
        S.dma(lambda e: e.dma_start(out=wr, in_=moe_w_router.rearrange("(k p) n -> p k n", p=128)), w=["rt_wr"], stream="rt_wr")
        low = B.sb([128, 128], F32)
        S.pool(lambda e: e.memset(low, 1.0), w=["rt_low"])
        S.pool(lambda e: e.affine_select(out=low, in_=low, pattern=[[1, 128]], compare_op=ALU.is_gt, fill=0.0, base=0, channel_multiplier=-1),
               r=["rt_low"], w=["rt_low"])
        hb = [B.sb([128, D], F32) for _ in range(2)]
        junk = B.sb([128, D], F32)
        xnb = [B.sb([128, D], BF16) for _ in range(2)]
        xn32 = B.sb([128, D], F32)
        u32T = B.sb([128, 8, 128], F32)
        st = B.sb([128, 8], F32)
        lg = B.sb([128, 8], F32)
        lg2 = B.sb([128, 8], F32)
        msum = B.sb([128, 8], F32)
        sm = B.sb([128, 8], F32)
        carry = B.sb([128, 8], F32)
        LG = B.sb([128, NC32, 8], F32)
        LG2 = B.sb([128, NC32, 8], F32)
        MS = B.sb([128, NC32, 8], F32)
        m1a = B.sb([128, NC32], F32)
        m2a = B.sb([128, NC32], F32)
        dma_ = B.sb([128, NC32], F32)
        w1a = B.sb([128, NC32], F32)
        S.pool(lambda e: e.memset(carry, 0.0), w=["rt_carry"])
        for c in range(NC32):
            b = c % 2
            h, xn = hb[b], xnb[b]
            kh, kx = "rt_h%d" % b, "rt_xn%d" % b
            S.dma(lambda e, h=h, c=c: e.dma_start(out=h, in_=out[c * 128:(c + 1) * 128, :]), r=["out"], w=[kh], stream=kh)
            ss, rs = st[:, 2 * b:2 * b + 1], st[:, 2 * b + 1:2 * b + 2]
            kss = "rt_ss%d" % b
            S.pool(lambda e, ss=ss: e.memset(ss, 0.0), w=[kss])
            S.act(lambda e, h=h, ss=ss: e.activation(out=junk, in_=h, func=AF.Square, accum_out=ss), r=[kh, kss], w=[kss, "rt_junk"])
            S.act(lambda e, ss=ss, rs=rs: e.activation(out=rs, in_=ss, func=AF.Ln, scale=1.0 / D, bias=B.epsb[:, 0:1]), r=[kss, "epsb"], w=[kss + "r"])
            S.act(lambda e, rs=rs: e.activation(out=rs, in_=rs, func=AF.Exp, scale=-0.5), r=[kss + "r"], w=[kss + "r"])
            S.dve(lambda e, h=h, xn=xn, rs=rs: e.tensor_scalar(out=xn, in0=h, scalar1=rs, scalar2=None, op0=ALU.mult), r=[kh, kss + "r"], w=[kx])
            S.dma(lambda e, xn=xn, c=c: e.dma_start(out=u_scr[c * 128:(c + 1) * 128, :], in_=xn), r=[kx], w=["u_scr"], stream=kx)
            S.dve(lambda e, h=h, rs=rs: e.tensor_scalar(out=xn32, in0=h, scalar1=rs, scalar2=None, op0=ALU.mult), r=[kh, kss + "r"], w=["rt_xn32"])
            for half in range(2):
                pb = 4 + half
                pT = B.bank(pb).rearrange("p (a b) -> p a b", a=4)
                for k in range(4):
                    kk = half * 4 + k
                    S.pe(lambda e, k=k, kk=kk, pT=pT: e.transpose(out=pT[:, k, :], in_=xn32[:, kk * 128:(kk + 1) * 128], identity=B.identf),
                         r=["rt_xn32", "identf"], w=["bank%d" % pb])
                S.dve(lambda e, half=half, pT=pT: e.tensor_tensor(out=u32T[:, half * 4:(half + 1) * 4, :], in0=pT,
                                                                  in1=nwm[:, half * 4:(half + 1) * 4].unsqueeze(2).to_broadcast([128, 4, 128]), op=ALU.mult),
                      r=["bank%d" % pb, "mo_nw"], w=["rt_u32T%d" % half])
            pl = B.bank(3, F32, 8)
            for k in range(8):
                S.pe(lambda e, k=k: e.matmul(pl, lhsT=u32T[:, k, :], rhs=wr[:, k, :], start=(k == 0), stop=(k == 7)),
                     r=["rt_u32T0", "rt_u32T1", "rt_wr"], w=["bank3"])
            S.dve(lambda e, c=c: e.tensor_copy(out=LG[:, c, :], in_=pl), r=["bank3"], w=["rt_LG%d" % c])
        allc = range(NC32)
        kLG = ["rt_LG%d" % c for c in allc]
        kM1 = ["rt_M1_%d" % c for c in allc]
        kM2 = ["rt_M2_%d" % c for c in allc]
        kW = ["rt_W12_%d" % c for c in allc]
        W12v = W12.rearrange("p (c k) -> p c k", k=2)

        def b8(ap):
            return ap.unsqueeze(2).to_broadcast([128, NC32, 8])
        S.dve(lambda e: e.reduce_max(out=m1a, in_=LG, axis=AX.X), r=kLG, w=["rt_m1a"])
        S.dve(lambda e: e.tensor_tensor(out=M1, in0=LG, in1=b8(m1a), op=ALU.is_equal), r=kLG + ["rt_m1a"], w=kM1)
        S.dve(lambda e: e.scalar_tensor_tensor(out=LG2, in0=M1, scalar=-1e30, in1=LG, op0=ALU.mult, op1=ALU.add), r=kM1 + kLG, w=["rt_LG2"])
        S.dve(lambda e: e.reduce_max(out=m2a, in_=LG2, axis=AX.X), r=["rt_LG2"], w=["rt_m2a"])
        S.dve(lambda e: e.tensor_tensor(out=M2, in0=LG2, in1=b8(m2a), op=ALU.is_equal), r=["rt_LG2", "rt_m2a"], w=kM2)
        S.dve(lambda e: e.tensor_tensor(out=dma_, in0=m2a, in1=m1a, op=ALU.subtract), r=["rt_m1a", "rt_m2a"], w=["rt_dma"])
        S.act(lambda e: e.activation(out=dma_, in_=dma_, func=AF.Exp), r=["rt_dma"], w=["rt_dma"])
        S.dve(lambda e: e.tensor_scalar(out=w1a, in0=dma_, scalar1=1.0, scalar2=None, op0=ALU.add), r=["rt_dma"], w=["rt_w1a"])
        S.dve(lambda e: e.reciprocal(out=w1a, in_=w1a), r=["rt_w1a"], w=["rt_w1a"])
        S.dve(lambda e: e.tensor_copy(out=W12v[:, :, 0], in_=w1a), r=["rt_w1a"], w=kW)
        S.dve(lambda e: e.tensor_tensor(out=W12v[:, :, 1], in0=dma_, in1=w1a, op=ALU.mult), r=["rt_dma", "rt_w1a"] + kW, w=kW)
        S.dve(lambda e: e.tensor_tensor(out=MS, in0=M1, in1=M2, op=ALU.add), r=kM1 + kM2, w=["rt_MS"])
        for c in range(NC32):
            pb = 2 + (c % 2)
            pp = B.bank(pb)
            S.pe(lambda e, c=c, pp=pp: e.matmul(pp[:, 0:8], lhsT=low, rhs=MS[:, c, :], start=True, stop=True), r=["rt_low", "rt_MS"], w=["bank%d" % pb])
            S.pe(lambda e, c=c, pp=pp: e.matmul(pp[:, 8:16], lhsT=B.ones, rhs=MS[:, c, :], start=True, stop=True), r=["ones", "rt_MS"], w=["bank%d" % pb])
            S.dve(lambda e, c=c, pp=pp: e.tensor_tensor(out=POS[:, c, :], in0=pp[:, 0:8], in1=carry, op=ALU.add), r=["bank%d" % pb, "rt_carry"], w=["rt_POS_%d" % c])
            S.dve(lambda e, pp=pp: e.tensor_tensor(out=carry, in0=pp[:, 8:16], in1=carry, op=ALU.add), r=["bank%d" % pb, "rt_carry"], w=["rt_carry"])
        tcnt = B.sb([128, 8], F32)
        rmod = B.sb([128, 8], F32)
        padded = B.sb([128, 8], F32)
        base = B.sb([128, 8], F32)
        bend = B.sb([128, 8], F32)
        tmp8 = B.sb([128, 8], F32)
        eu = B.sb([128, NU], F32)
        S.dve(lambda e: e.tensor_scalar(out=tcnt, in0=carry, scalar1=float(UT - 1), scalar2=None, op0=ALU.add), r=["rt_carry"], w=["rt_tcnt"])
        ri = B.sb([128, 8], I32)
        S.dve(lambda e: e.tensor_scalar(out=tcnt, in0=tcnt, scalar1=1.0 / UT, scalar2=None, op0=ALU.mult), r=["rt_tcnt"], w=["rt_tcnt"])
        S.dve(lambda e: e.tensor_copy(out=ri, in_=tcnt), r=["rt_tcnt"], w=["rt_ri"])
        S.dve(lambda e: e.tensor_copy(out=rmod, in_=ri), r=["rt_ri"], w=["rt_rmod"])
        S.dve(lambda e: e.tensor_tensor(out=padded, in0=rmod, in1=tcnt, op=ALU.is_gt), r=["rt_tcnt", "rt_rmod"], w=["rt_padded"])
        S.dve(lambda e: e.tensor_tensor(out=padded, in0=rmod, in1=padded, op=ALU.subtract), r=["rt_rmod", "rt_padded"], w=["rt_padded"])
        S.dve(lambda e: e.tensor_scalar(out=padded, in0=padded, scalar1=float(UT), scalar2=None, op0=ALU.mult), r=["rt_padded"], w=["rt_padded"])
        S.pool(lambda e: e.memset(base, 0.0), w=["rt_base"])
        for ei in range(1, 8):
            S.dve(lambda e, ei=ei: e.tensor_tensor(out=base[:, ei:ei + 1], in0=base[:, ei - 1:ei], in1=padded[:, ei - 1:ei], op=ALU.add),
                  r=["rt_base", "rt_padded"], w=["rt_base"])
        S.dve(lambda e: e.tensor_tensor(out=bend, in0=base, in1=padded, op=ALU.add), r=["rt_base", "rt_padded"], w=["rt_bend"])
        for c in range(NC32):
            for k, MM in enumerate((M1, M2)):
                S.dve(lambda e, c=c: e.tensor_tensor(out=tmp8, in0=POS[:, c, :], in1=base, op=ALU.add), r=["rt_POS_%d" % c, "rt_base"], w=["rt_tmp8"])
                S.dve(lambda e, c=c, MM=MM: e.tensor_tensor(out=tmp8, in0=tmp8, in1=MM[:, c, :], op=ALU.mult), r=["rt_tmp8", "rt_M%d_%d" % (k + 1, c)], w=["rt_tmp8"])
                S.dve(lambda e, c=c, k=k: e.reduce_sum(out=slots_f[:, 2 * c + k:2 * c + k + 1], in_=tmp8, axis=AX.X), r=["rt_tmp8"], w=["rt_slots_f"])
        S.dve(lambda e: e.tensor_copy(out=slots_i, in_=slots_f), r=["rt_slots_f"], w=["mo_slots"])
        for u in range(NU):
            S.dve(lambda e, u=u: e.tensor_scalar(out=tmp8, in0=bend, scalar1=float(UT * u), scalar2=None, op0=ALU.is_le), r=["rt_bend"], w=["rt_tmp8"])
            S.dve(lambda e, u=u: e.reduce_sum(out=eu[:, u:u + 1], in_=tmp8, axis=AX.X), r=["rt_tmp8"], w=["rt_eu"])
        S.dve(lambda e: e.tensor_scalar(out=eu, in0=eu, scalar1=7.0, scalar2=None, op0=ALU.min), r=["rt_eu"], w=["rt_eu"])
        ioi = B.sb([128, 28], I32)
        iof = B.sb([128, 28], F32)
        eus = B.sb([128, NU], F32)
        idxg_f = B.sb([128, NU * 8], F32)
        idxd_f = B.sb([128, NU * 28], F32)
        S.pool(lambda e: e.iota(ioi, pattern=[[128, 28]], base=0, channel_multiplier=1), w=["rt_ioi"])
        S.dve(lambda e: e.tensor_copy(out=iof, in_=ioi), r=["rt_ioi"], w=["rt_iof"])
        S.dve(lambda e: e.tensor_scalar(out=eus, in0=eu, scalar1=float(D), scalar2=None, op0=ALU.mult), r=["rt_eu"], w=["rt_eus"])
        for u in range(NU):
            S.dve(lambda e, u=u: e.tensor_scalar(out=idxg_f[:, u * 8:(u + 1) * 8], in0=iof[:, 0:8], scalar1=eus[:, u:u + 1], scalar2=None, op0=ALU.add),
                  r=["rt_iof", "rt_eus"], w=["rt_idxg_f"])
        tmpi = B.sb([128, NU * 28], F32)
        for ft in range(7):
            S.dve(lambda e, ft=ft: e.tensor_scalar(out=tmpi[:, 0:NU * 8], in0=idxg_f, scalar1=7.0, scalar2=float(ft), op0=ALU.mult, op1=ALU.add),
                  r=["rt_idxg_f"], w=["rt_tmpi"])
            S.dve(lambda e, ft=ft: e.tensor_copy(out=idxg[:, ft, :], in_=tmpi[:, 0:NU * 8]), r=["rt_tmpi"], w=["mo_idxg"])
        S.dve(lambda e: e.tensor_scalar(out=eus, in0=eu, scalar1=float(DFF), scalar2=None, op0=ALU.mult), r=["rt_eu", "rt_idxg_f"], w=["rt_eus"])
        for u in range(NU):
            S.dve(lambda e, u=u: e.tensor_scalar(out=idxd_f[:, u * 28:(u + 1) * 28], in0=iof, scalar1=eus[:, u:u + 1], scalar2=None, op0=ALU.add),
                  r=["rt_iof", "rt_eus"], w=["rt_idxd_f"])
        for ti in range(2):
            S.dve(lambda e, ti=ti: e.tensor_scalar(out=tmpi, in0=idxd_f, scalar1=2.0, scalar2=float(ti), op0=ALU.mult, op1=ALU.add),
                  r=["rt_idxd_f"], w=["rt_tmpi"])
            S.dve(lambda e, ti=ti: e.tensor_copy(out=idxd[:, ti, :], in_=tmpi), r=["rt_tmpi"], w=["mo_idxd"])
        zi = B.sb([128, NSLOT // 128], I32)
        tokid = B.sb([128, NC32], I32)
        S.pool(lambda e: e.memset(zi, 0), w=["rt_zi"])
        S.pool(lambda e: e.iota(tokid, pattern=[[128, NC32]], base=0, channel_multiplier=1), w=["rt_tokid"])
        S.dma(lambda e: e.dma_start(out=slot_tok.rearrange("(p a) o -> p (a o)", p=128), in_=zi), r=["rt_zi"], w=["slot_tok"], stream="rt_zi")
        for c in range(NC32):
            for k in range(2):
                S.dma(lambda e, c=c, k=k: e.indirect_dma_start(out=slot_tok, out_offset=bass.IndirectOffsetOnAxis(ap=slots_i[:, 2 * c + k:2 * c + k + 1], axis=0),
                                                               in_=tokid[:, c:c + 1], in_offset=None),
                      r=["mo_slots", "rt_tokid"], w=["slot_tok"], q="pool", stream="rt_sct")
        if B.debug:
            B.dbg("slots_i", slots_i, ["mo_slots"])
            B.dbg("W12", W12, ["rt_W12_%d" % c for c in range(NC32)])
            B.dbg("eu", eu, ["rt_eu"])
            B.dbg("idxg", idxg, ["mo_idxg"])
            B.dbg("idxd", idxd, ["mo_idxd"])

    stage_route()

    B.reset_arena(pmark)
    mu_uT = [B.sb([128, 8, UT], BF16) for _ in range(2)]
    mu_actT = B.sb([128, 28, UT], BF16)
    mu_acc = [B.sb([128, UNTC, D], F32) for _ in range(2)]
    mu_wgu = [B.sb([128, 8, 1024], BF16) for _ in range(2)]
    mu_wb28 = [(B.sb([128, 28, 512], BF16), "mu_wb28_%d" % i) for i in range(2)]
    mu_sg = [B.sb([128, 512], F32) for _ in range(2)]
    mu_xg = [B.sb([128, D], BF16) for _ in range(2)]
    mu_sidx = [B.sb([128, UNTC], I32) for _ in range(2)]

    def unit_pre(u):
        par = u % 2
        r0 = u * UT
        uT, sidx = mu_uT[par], mu_sidx[par]
        ksi = "mu_sidx%d" % par
        S.dma(lambda e: e.dma_start(out=sidx, in_=slot_tok[r0:r0 + UT, :].rearrange("(c p) o -> p (c o)", p=128), allow_slow_non_contiguous=True),
              r=["slot_tok"], w=[ksi], stream=ksi)
        ukeys = ["mu_uT%d_%d" % (par, c) for c in range(UNTC)]
        for c in range(UNTC):
            b = c % 2
            kx = "mu_xg%d" % b
            S.dma(lambda e, c=c, b=b: e.indirect_dma_start(out=mu_xg[b], out_offset=None, in_=u_scr, in_offset=bass.IndirectOffsetOnAxis(ap=sidx[:, c:c + 1], axis=0)),
                  r=[ksi, "u_scr"], w=[kx], q="pool", stream=kx)
            pb = 6 + b
            pT = B.bank(pb, BF16).rearrange("p (a b) -> p a b", a=8)
            for k in range(8):
                S.pe(lambda e, k=k, b=b, pT=pT: e.transpose(out=pT[:, k, :], in_=mu_xg[b][:, k * 128:(k + 1) * 128], identity=B.ident), r=[kx, "ident"], w=["bank%d" % pb])
            S.dve(lambda e, c=c, pT=pT: e.tensor_tensor(out=uT[:, :, c * 128:(c + 1) * 128], in0=pT, in1=nwm.unsqueeze(2).to_broadcast([128, 8, 128]), op=ALU.mult),
                  r=["bank%d" % pb, "mo_nw"], w=[ukeys[c]])

    def unit_gateup(u):
        par = u % 2
        uT = mu_uT[par]
        actT = mu_actT
        ukeys = ["mu_uT%d_%d" % (par, c) for c in range(UNTC)]
        for ft in range(7):
            wgu = mu_wgu[ft % 2]
            wb_g, wb_u = wgu[:, :, 0:512], wgu[:, :, 512:1024]
            for k in range(8):
                kk = "mu_wgu%d_%d" % (ft % 2, k)
                S.dma(lambda e, wgu=wgu, k=k, ft=ft: e.indirect_dma_start(
                    out=wgu[:, k, :], out_offset=None, in_=wgu2d,
                    in_offset=bass.IndirectOffsetOnAxis(ap=idxg[:, ft, u * 8 + k:u * 8 + k + 1], axis=0)),
                    r=["mo_idxg", "cv_g", "cv_u"], w=[kk], q="pool", stream=kk)
            for j in range(4):
                fc = ft * 4 + j
                pg = B.bank(fc % 2)
                pu = B.bank(2 + fc % 2)
                for k in range(8):
                    S.pe(lambda e, k=k, j=j, pg=pg, wb_g=wb_g: e.matmul(pg, lhsT=wb_g[:, k, j * 128:(j + 1) * 128], rhs=uT[:, k, :], start=(k == 0), stop=(k == 7)),
                         r=["mu_wgu%d_%d" % (ft % 2, k)] + ukeys, w=["bank%d" % (fc % 2)])
                for k in range(8):
                    S.pe(lambda e, k=k, j=j, pu=pu, wb_u=wb_u: e.matmul(pu, lhsT=wb_u[:, k, j * 128:(j + 1) * 128], rhs=uT[:, k, :], start=(k == 0), stop=(k == 7)),
                         r=["mu_wgu%d_%d" % (ft % 2, k)] + ukeys, w=["bank%d" % (2 + fc % 2)])
                sg_ = mu_sg[fc % 2]
                ksg = "mu_sg%d" % (fc % 2)
                S.act(lambda e, pg=pg, sg_=sg_: e.activation(out=sg_, in_=pg, func=AF.Silu), r=["bank%d" % (fc % 2)], w=[ksg])
                S.dve(lambda e, fc=fc, pu=pu, sg_=sg_: e.tensor_tensor(out=actT[:, fc, :], in0=sg_, in1=pu, op=ALU.mult), r=[ksg, "bank%d" % (2 + fc % 2)], w=["mu_actT"])

    def unit_down(u):
        tag = "mu"
        par = u % 2
        r0 = u * UT
        acc = mu_acc[par]
        actT = mu_actT
        akey = lambda c: "mu_actT"
        for ti4 in range(2):
            def ep_down(ti, c, ps, kp, ti4=ti4):
                S.act(lambda e: e.activation(out=acc[:, c, ti4 * 512:(ti4 + 1) * 512], in_=ps, func=AF.Identity), r=[kp], w=["mu_acc%d_%d" % (par, c)])
            idn = (idxd[:, ti4, u * 28:(u + 1) * 28], "mo_idxd")
            B.linear_tok(actT, 28, UNTC, wd2d, [(0, 512)], ep_down, tag + "d", akey, mu_wb28, banks=(4, 5), widx=idn)
        S.dma(lambda e: e.dma_start(out=y_slots[r0:r0 + UT, :].rearrange("(c p) n -> p c n", p=128), in_=acc), r=["mu_acc%d_%d" % (par, c) for c in range(UNTC)], w=["y_slots"],
              stream="mu_accst%d" % par)

    unit_pre(0)
    for u in range(NU):
        unit_gateup(u)
        if u + 1 < NU:
            unit_pre(u + 1)
        unit_down(u)

    def stage_combine():
        tag = "cb"
        B.reset_arena(pmark)
        hb = [B.sb([128, D], F32) for _ in range(2)]
        y1 = [B.sb([128, D], F32) for _ in range(2)]
        y2 = [B.sb([128, D], F32) for _ in range(2)]
        for c in range(NC32):
            b = c % 2
            kh, k1, k2 = "cb_h%d" % b, "cb_y1%d" % b, "cb_y2%d" % b
            S.dma(lambda e, b=b, c=c: e.dma_start(out=hb[b], in_=out[c * 128:(c + 1) * 128, :]), r=["out"], w=[kh], stream=kh)
            S.dma(lambda e, b=b, c=c: e.indirect_dma_start(out=y1[b], out_offset=None, in_=y_slots, in_offset=bass.IndirectOffsetOnAxis(ap=slots_i[:, 2 * c:2 * c + 1], axis=0)),
                  r=["y_slots", "mo_slots"], w=[k1], q="pool", stream=k1)
            S.dma(lambda e, b=b, c=c: e.indirect_dma_start(out=y2[b], out_offset=None, in_=y_slots, in_offset=bass.IndirectOffsetOnAxis(ap=slots_i[:, 2 * c + 1:2 * c + 2], axis=0)),
                  r=["y_slots", "mo_slots"], w=[k2], q="pool", stream=k2)
            S.dve(lambda e, b=b, c=c: e.scalar_tensor_tensor(out=hb[b], in0=y1[b], scalar=W12[:, 2 * c:2 * c + 1], in1=hb[b], op0=ALU.mult, op1=ALU.add),
                  r=[k1, kh, "rt_W12_%d" % c], w=[kh])
            S.dve(lambda e, b=b, c=c: e.scalar_tensor_tensor(out=hb[b], in0=y2[b], scalar=W12[:, 2 * c + 1:2 * c + 2], in1=hb[b], op0=ALU.mult, op1=ALU.add),
                  r=[k2, kh, "rt_W12_%d" % c], w=[kh])
            S.dma(lambda e, b=b, c=c: e.dma_start(out=out[c * 128:(c + 1) * 128, :], in_=hb[b]), r=[kh], w=["out"], stream=kh)

    stage_combine()
    if stop_after == "moe":
        S.finish()
        return B
    for tile in range(TOK // TT):
        stage_ple(tile, 1)

    S.finish()
    return B


def _rep(v, n=128):
    return np.ascontiguousarray(np.broadcast_to(np.asarray(v, np.float32)[None], (n,) + tuple(np.shape(v))))


def _col(v):
    v = np.asarray(v, np.float32)
    return np.ascontiguousarray(v.reshape(-1, 128).T)


def prep_inputs(inp, core):
    b0 = core * NSEQ
    m = {}
    m["x"] = np.ascontiguousarray(inp["x"][b0:b0 + NSEQ].reshape(TOK, D))
    m["p"] = np.ascontiguousarray(inp["p"][:, b0:b0 + NSEQ].reshape(2, TOK, 256))
    m["ssm_norm_w"] = _col(inp["ssm_norm_w"][0])
    m["ssm_w_in"] = np.ascontiguousarray(inp["ssm_w_in"][0])
    m["conv_w"] = np.ascontiguousarray(np.broadcast_to(np.asarray(inp["ssm_conv_w"][0], np.float32)[:, None, :], (4, 128, 3072)))
    m["conv_b"] = _rep(inp["ssm_conv_b"][0])
    m["dt_bias"] = _rep(inp["ssm_dt_bias"][0])
    m["a_log"] = _rep(inp["ssm_a_log"][0])
    m["d_skip"] = _rep(inp["ssm_d"][0])
    m["gn_w"] = _col(inp["ssm_gn_w"][0])
    m["ssm_w_out"] = np.ascontiguousarray(inp["ssm_w_out"][0])
    m["ffn_norm_w"] = _col(inp["ffn_norm_w"][0])
    m["moe_norm_w"] = _col(inp["moe_norm_w"][0])
    m["moe_w_router"] = np.ascontiguousarray(inp["moe_w_router"][0])
    m["moe_w_gate"] = np.ascontiguousarray(inp["moe_w_gate"][0])
    m["moe_w_up"] = np.ascontiguousarray(inp["moe_w_up"][0])
    m["moe_w_down"] = np.ascontiguousarray(inp["moe_w_down"][0])
    m["kv_norm_w"] = _col(inp["kv_norm_w"])
    m["w_kv"] = np.ascontiguousarray(inp["w_kv"])
    m["b_f"] = _rep(inp["b_f"])
    m["k_norm_w"] = _rep(inp["k_norm_w"])
    m["att_norm_w"] = _col(inp["att_norm_w"][0])
    m["att_w_q"] = np.ascontiguousarray(inp["att_w_q"][0])
    m["q_norm_w"] = _rep(inp["q_norm_w"][0])
    m["att_w_o"] = np.ascontiguousarray(inp["att_w_o"][0])
    m["ple_norm_w"] = np.stack([_col(inp["ple_norm_w"][i]) for i in range(2)])
    m["ple_w_gate"] = np.ascontiguousarray(inp["ple_w_gate"])
    m["ple_w_proj"] = np.ascontiguousarray(inp["ple_w_proj"])
    m["ffn_w_gate"] = np.ascontiguousarray(inp["ffn_w_gate"][0])
    m["ffn_w_up"] = np.ascontiguousarray(inp["ffn_w_up"][0])
    m["ffn_w_down"] = np.ascontiguousarray(inp["ffn_w_down"][0])
    return m


def kernel(**inputs):
    inp = {k: np.asarray(v) for k, v in inputs.items()}
    B = build_program()
    in_maps = []
    for c in range(NCORES):
        m = prep_inputs(inp, c)
        in_maps.append({k: v for k, v in m.items() if k in B.inputs})
    res = run_bass_kernel_spmd(B.nc, in_maps, core_ids=list(range(NCORES)))
    outs = [np.asarray(r["out"]).reshape(NSEQ, L, D) for r in res.results]
    return np.concatenate(outs, axis=0).astype(np.float32)
```
